# Optimizing a Trainium2 kernel written in Bass

```python
import math
import jax, jax.numpy as jnp
from jax import lax
import numpy as np

D_MODEL = 1024
BATCH = 1
SEQ = 16384
DEPTH = 1

MIX_WIDTH = D_MODEL
HGRN_WIDTH = MIX_WIDTH // 2
HGRN_HEADS = 4
HGRN_HEAD_DIM = HGRN_WIDTH // HGRN_HEADS
HGRN_CHUNK = 64
POOL_WIDTH = MIX_WIDTH - HGRN_WIDTH
POOL_WINDOWS = (2, 4, 8, 16)
POOL_GROUP = POOL_WIDTH // len(POOL_WINDOWS)
IN_PROJ_WIDTH = 4 * HGRN_WIDTH + POOL_WIDTH
N_MEM = 256
XATTN_HEADS = 4
XATTN_HEAD_DIM = D_MODEL // XATTN_HEADS
N_EXPERTS = 32
TOP_K = 4
D_EXPERT = D_MODEL
SWIGLU_LIMIT = 7.0
SWIGLU_ALPHA = 1.702
MOE_BLOCK = 256
DEEPNORM_ALPHA = (2.0 * DEPTH) ** 0.25
DEEPNORM_BETA = (8.0 * DEPTH) ** -0.25
LN_EPS = 1e-5
RMS_EPS = 1e-6

kernel_name = "hgrn2_pool_memxattn_moe_deepnorm"


def layer_norm(x, w, b):
    xf = x.astype(jnp.float32)
    mu = jnp.mean(xf, axis=-1, keepdims=True)
    var = jnp.mean(jnp.square(xf - mu), axis=-1, keepdims=True)
    return ((xf - mu) * lax.rsqrt(var + LN_EPS) * w.astype(jnp.float32) + b.astype(jnp.float32)).astype(x.dtype)


def hgrn2_mixer(q, f_logit, v, g, lb, norm_w):
    B, S, _ = q.shape
    H, Dh, C = HGRN_HEADS, HGRN_HEAD_DIM, HGRN_CHUNK
    nc = S // C
    f32 = jnp.float32
    lb = lb.astype(f32)
    zf = f_logit.astype(f32)
    log_f = jnp.log(lb + (1.0 - lb) * jax.nn.sigmoid(zf))
    key = (1.0 - lb) * jax.nn.sigmoid(-zf)

    def to_chunks(t):
        return t.astype(f32).reshape(B, nc, C, H, Dh).transpose(1, 0, 3, 2, 4)

    causal = jnp.tril(jnp.ones((C, C), dtype=bool))

    def step(state, inp):
        qc, kc, vc, lfc = inp
        b = jnp.cumsum(lfc, axis=-2)
        o_inter = jnp.einsum('bhtk,bhkv->bhtv', qc * jnp.exp(b), state)
        diff = b[:, :, :, None, :] - b[:, :, None, :, :]
        decay = jnp.where(causal[:, :, None], jnp.exp(jnp.minimum(diff, 0.0)), 0.0)
        scores = jnp.einsum('bhtk,bhtsk,bhsk->bhts', qc, decay, kc)
        o_intra = jnp.einsum('bhts,bhsv->bhtv', scores, vc)
        b_last = b[:, :, -1:, :]
        new_state = (jnp.exp(b_last[:, :, 0, :])[..., None] * state
                     + jnp.einsum('bhsk,bhsv->bhkv', kc * jnp.exp(b_last - b), vc))
        return new_state, o_inter + o_intra

    init = jnp.zeros((B, H, Dh, Dh), f32)
    _, o = lax.scan(step, init, (to_chunks(q), to_chunks(key), to_chunks(v), to_chunks(log_f)))
    o = o.transpose(1, 0, 3, 2, 4).reshape(B, S, H, Dh)
    o = o * lax.rsqrt(jnp.mean(jnp.square(o), axis=-1, keepdims=True) + RMS_EPS) \
        * norm_w.astype(f32).reshape(H, Dh)
    gate = jax.nn.silu(g.astype(f32)).reshape(B, S, H, Dh)
    return (o * gate).reshape(B, S, H * Dh)


def pool_mixer(p, w_pool, scale):
    B, S, _ = p.shape
    pf = p.astype(jnp.float32)
    pos = jnp.arange(S)
    outs = []
    for gi, w in enumerate(POOL_WINDOWS):
        pg = pf[..., gi * POOL_GROUP:(gi + 1) * POOL_GROUP]
        cs = jnp.cumsum(pg, axis=1)
        cs_shift = jnp.pad(cs, ((0, 0), (w, 0), (0, 0)))[:, :S]
        count = jnp.minimum(pos + 1, w).astype(jnp.float32)
        mean = (cs - cs_shift) / count[None, :, None]
        outs.append(jnp.einsum('bsc,cd->bsd', mean - pg, w_pool[gi].astype(jnp.float32)))
    return jnp.concatenate(outs, axis=-1) * scale.astype(jnp.float32)


def memory_cross_attention(x, mem, wq, wk, wv, wo):
    B, S, D = x.shape
    M = mem.shape[1]
    q = (x @ wq).reshape(B, S, XATTN_HEADS, XATTN_HEAD_DIM)
    k = (mem @ wk).reshape(B, M, XATTN_HEADS, XATTN_HEAD_DIM)
    v = (mem @ wv).reshape(B, M, XATTN_HEADS, XATTN_HEAD_DIM)
    s = jnp.einsum('bshd,bmhd->bhsm', q, k).astype(jnp.float32) * (XATTN_HEAD_DIM ** -0.5)
    pr = jax.nn.softmax(s, axis=-1).astype(v.dtype)
    o = jnp.einsum('bhsm,bmhd->bshd', pr, v).reshape(B, S, D)
    return o @ wo


def clamped_swiglu(h):
    h_glu, h_lin = h[..., :D_EXPERT], h[..., D_EXPERT:]
    h_glu = jnp.minimum(h_glu, SWIGLU_LIMIT)
    h_lin = jnp.clip(h_lin, -SWIGLU_LIMIT, SWIGLU_LIMIT)
    return h_glu * jax.nn.sigmoid(SWIGLU_ALPHA * h_glu) * (h_lin + 1.0)


def moe_ffn(x, w_r, b_r, w1, b1, w2, b2):
    B, S, D = x.shape
    T = B * S
    TK = T * TOP_K
    xt = x.reshape(T, D)
    logits = (xt @ w_r + b_r).astype(jnp.float32)
    top_v, top_i = lax.top_k(logits, TOP_K)
    gates = jax.nn.softmax(top_v, axis=-1)
    e_flat = top_i.reshape(-1).astype(jnp.int32)
    tok_flat = jnp.repeat(jnp.arange(T, dtype=jnp.int32), TOP_K)
    g_flat = gates.reshape(-1)
    order = jnp.argsort(e_flat)
    e_sorted, tok_sorted, g_sorted = e_flat[order], tok_flat[order], g_flat[order]
    counts = jnp.zeros((N_EXPERTS,), jnp.int32).at[e_flat].add(1)
    padded = ((counts + MOE_BLOCK - 1) // MOE_BLOCK) * MOE_BLOCK
    start = jnp.cumsum(counts) - counts
    pend = jnp.cumsum(padded)
    pstart = pend - padded
    rank = jnp.arange(TK, dtype=jnp.int32) - start[e_sorted]
    dest = pstart[e_sorted] + rank
    n_blocks = -(-(TK + N_EXPERTS * (MOE_BLOCK - 1)) // MOE_BLOCK)
    L = n_blocks * MOE_BLOCK
    buf_tok = jnp.zeros((L,), jnp.int32).at[dest].set(tok_sorted)
    buf_gate = jnp.zeros((L,), jnp.float32).at[dest].set(g_sorted)
    block_start = jnp.arange(n_blocks, dtype=jnp.int32) * MOE_BLOCK
    block_e = jnp.clip(jnp.searchsorted(pend, block_start, side='right'), 0, N_EXPERTS - 1)

    def block_fn(args):
        tok, e = args
        xb = xt[tok]
        h = xb @ w1[e] + b1[e]
        return clamped_swiglu(h) @ w2[e] + b2[e]

    outs = lax.map(block_fn, (buf_tok.reshape(n_blocks, MOE_BLOCK), block_e))
    contrib = outs.reshape(L, D).astype(jnp.float32) * buf_gate[:, None]
    y = jnp.zeros((T, D), jnp.float32).at[buf_tok].add(contrib)
    return y.reshape(B, S, D).astype(x.dtype)


def setup_inputs(seed: int = 0) -> dict:
    key = jax.random.key(seed)
    ks = jax.random.split(key, 24)
    f32 = jnp.float32
    nrm = lambda k, shape, s: jax.random.normal(k, shape, f32) * s
    L, D, E, F = DEPTH, D_MODEL, N_EXPERTS, D_EXPERT
    return {
        "x": nrm(ks[0], (BATCH, SEQ, D), 1.0),
        "mem": nrm(ks[1], (BATCH, N_MEM, D), 1.0),
        "w_in": nrm(ks[2], (L, D, IN_PROJ_WIDTH), D ** -0.5),
        "lb_logits": nrm(ks[3], (L + 1, HGRN_WIDTH), 1.0),
        "hgrn_norm_w": 1.0 + nrm(ks[4], (L, HGRN_WIDTH), 0.02),
        "w_pool": nrm(ks[5], (L, len(POOL_WINDOWS), POOL_GROUP, POOL_GROUP), POOL_GROUP ** -0.5),
        "pool_scale": 1.0 + nrm(ks[6], (L, POOL_WIDTH), 0.1),
        "w_out": nrm(ks[7], (L, MIX_WIDTH, D), MIX_WIDTH ** -0.5 * DEEPNORM_BETA),
        "ln1_w": 1.0 + nrm(ks[8], (L, D), 0.02),
        "ln1_b": nrm(ks[9], (L, D), 0.02),
        "w_xq": nrm(ks[10], (L, D, D), D ** -0.5),
        "w_xk": nrm(ks[11], (L, D, D), D ** -0.5),
        "w_xv": nrm(ks[12], (L, D, D), D ** -0.5 * DEEPNORM_BETA),
        "w_xo": nrm(ks[13], (L, D, D), D ** -0.5 * DEEPNORM_BETA),
        "ln2_w": 1.0 + nrm(ks[14], (L, D), 0.02),
        "ln2_b": nrm(ks[15], (L, D), 0.02),
        "w_router": nrm(ks[16], (L, D, E), D ** -0.5),
        "b_router": nrm(ks[17], (L, E), 0.01),
        "w1": nrm(ks[18], (L, E, D, 2 * F), D ** -0.5),
        "b1": nrm(ks[19], (L, E, 2 * F), 0.02),
        "w2": nrm(ks[20], (L, E, F, D), F ** -0.5 * DEEPNORM_BETA),
        "b2": nrm(ks[21], (L, E, D), 0.02),
        "ln3_w": 1.0 + nrm(ks[22], (L, D), 0.02),
        "ln3_b": nrm(ks[23], (L, D), 0.02),
    }


def reference(x, mem, w_in, lb_logits, hgrn_norm_w, w_pool, pool_scale, w_out, ln1_w, ln1_b,
              w_xq, w_xk, w_xv, w_xo, ln2_w, ln2_b, w_router, b_router, w1, b1, w2, b2,
              ln3_w, ln3_b):
    W = HGRN_WIDTH
    lower_bounds = jnp.cumsum(jax.nn.softmax(lb_logits.astype(jnp.float32), axis=0), axis=0)
    for l in range(DEPTH):
        proj = x @ w_in[l]
        q, f_logit, i_in, g = proj[..., :W], proj[..., W:2 * W], proj[..., 2 * W:3 * W], proj[..., 3 * W:4 * W]
        p_in = proj[..., 4 * W:]
        h_rec = hgrn2_mixer(q, f_logit, i_in, g, lower_bounds[l], hgrn_norm_w[l])
        h_pool = pool_mixer(p_in, w_pool[l], pool_scale[l])
        mix = jnp.concatenate([h_rec, h_pool], axis=-1).astype(x.dtype) @ w_out[l]
        x = layer_norm(DEEPNORM_ALPHA * x + mix, ln1_w[l], ln1_b[l])
        xa = memory_cross_attention(x, mem, w_xq[l], w_xk[l], w_xv[l], w_xo[l])
        x = layer_norm(DEEPNORM_ALPHA * x + xa, ln2_w[l], ln2_b[l])
        y = moe_ffn(x, w_router[l], b_router[l], w1[l], b1[l], w2[l], b2[l])
        x = layer_norm(DEEPNORM_ALPHA * x + y, ln3_w[l], ln3_b[l])
    return x
```

```python
import os
from contextlib import ExitStack
import numpy as np
import concourse.bass as bass
import concourse.mybir as mybir
from concourse.bass_utils import run_bass_kernel_spmd

F32 = mybir.dt.float32
F32R = mybir.dt.float32r
I32 = mybir.dt.int32
U32 = mybir.dt.uint32
ALU = mybir.AluOpType
AF = mybir.ActivationFunctionType
AX = mybir.AxisListType


import types


def _snap(fn):
    if fn.__closure__ is None:
        return fn
    cells = []
    for c in fn.__closure__:
        try:
            cells.append(types.CellType(c.cell_contents))
        except ValueError:
            cells.append(c)
    return types.FunctionType(fn.__code__, fn.__globals__, fn.__name__, fn.__defaults__, tuple(cells))


class Buf:
    __slots__ = ("name", "w", "r", "dsem", "dcnt", "excl")

    def __init__(self, name):
        self.name = name
        self.excl = False
        self.w = None
        self.r = []
        self.dsem = None
        self.dcnt = 0


class Eng:
    def __init__(self, name, sem):
        self.name = name
        self.sem = sem
        self.cnt = 0
        self.ops = []
        self.known = {}


class Prog:
    def __init__(self, nc, stack):
        self.nc = nc
        self.stack = stack
        self.engs = {}
        self.bufs = []
        self.nsem = 0
        for n in ("pe", "act", "dve", "pool", "sp"):
            self.engs[n] = Eng(n, self.sem("e_" + n))
        self.sb_off = 0
        self.arena = None
        self.sb_marks = []
        self.uid = 0

    def sem(self, name):
        self.nsem += 1
        return self.stack.enter_context(self.nc.semaphore(name))

    def buf(self, name="b"):
        b = Buf(name)
        self.bufs.append(b)
        return b

    def sb(self, shape, dt=F32, name=None):
        if self.arena is None:
            nbytes = self.nc.sbuf_bytes_remaining
            self.arena_cols = (nbytes // 4) - 64
            self.arena = self.stack.enter_context(self.nc.sbuf_tensor("arena", [128, self.arena_cols], F32))
        n = 1
        for s_ in shape[1:]:
            n *= s_
        if dt not in (F32, F32R, I32, U32):
            n = (n + 1) // 2
        n = (n + 15) // 16 * 16
        off = self.sb_off
        self.sb_off += n
        assert self.sb_off <= self.arena_cols, ("SBUF overflow", self.sb_off, self.arena_cols)
        ap = self.arena[0:shape[0], off:off + n]
        if dt != F32:
            ap = ap.bitcast(dt)
        if len(shape) == 2:
            ap = ap[:, 0:shape[1]]
        elif len(shape) == 3:
            ap = ap[:, 0:shape[1] * shape[2]].rearrange("p (a b) -> p a b", a=shape[1], b=shape[2])
        else:
            ap = ap[:, 0:shape[1] * shape[2] * shape[3]].rearrange("p (a b c) -> p a b c", a=shape[1], b=shape[2], c=shape[3])
        return ap

    def mark(self):
        self.sb_marks.append(self.sb_off)

    def release(self):
        self.sb_off = self.sb_marks.pop()

    def _deps(self, E, reads, writes):
        deps = {}

        def add(tok):
            if tok is None:
                return
            s, v = tok
            k = id(s)
            if k not in deps or deps[k][1] < v:
                deps[k] = (s, v)

        for b in reads:
            add(b.w)
            if b.excl:
                for r in b.r:
                    if r[0] is not E.sem:
                        add(r)
        for b in writes:
            add(b.w)
            for r in b.r:
                add(r)
        for k, (s, v) in deps.items():
            if E.name == "pe" and s is E.sem:
                continue
            if E.known.get(k, 0) < v:
                E.ops.append(("w", s, v))
                E.known[k] = v

    def op(self, en, fn, reads=(), writes=()):
        E = self.engs[en]
        self._deps(E, reads, writes)
        E.cnt += 1
        E.ops.append(("o", _snap(fn), E.sem, 1))
        tok = (E.sem, E.cnt)
        for b in writes:
            b.w = tok
            b.r = []
        for b in reads:
            if b.w is not tok:
                b.r.append(tok)
        return tok

    def dma(self, en, fn, reads=(), writes=(), chain=True):
        E = self.engs[en]
        D = writes[0]
        if not chain:
            saved = D.w
            D.w = None
            self._deps(E, reads, writes)
            D.w = saved
        else:
            self._deps(E, reads, writes)
        if D.dsem is None:
            D.dsem = self.sem("d_" + D.name)
        D.dcnt += 16
        E.ops.append(("o", _snap(fn), D.dsem, 16))
        tok = (D.dsem, D.dcnt)
        for b in writes:
            b.w = tok
            b.r = []
        for b in reads:
            b.r.append(tok)
        return tok

    def barrier(self):
        toks = [(E.sem, E.cnt) for E in self.engs.values() if E.cnt > 0]
        toks += [(b.dsem, b.dcnt) for b in self.bufs if b.dsem is not None]
        for E in self.engs.values():
            for s, v in toks:
                if E.name == "pe" and s is E.sem:
                    continue
                if s is E.sem and E.name == "sp":
                    continue
                k = id(s)
                if E.known.get(k, 0) < v:
                    E.ops.append(("w", s, v))
                    E.known[k] = v
        for b in self.bufs:
            b.r = []

    def emit(self):
        with self.nc.Block() as block:
            def mk(E):
                def body(eng):
                    for o in E.ops:
                        if o[0] == "w":
                            eng.wait_ge(o[1], o[2])
                        else:
                            o[1](eng).then_inc(o[2], o[3])
                return body

            block.tensor(mk(self.engs["pe"]))
            block.scalar(mk(self.engs["act"]))
            block.vector(mk(self.engs["dve"]))
            block.gpsimd(mk(self.engs["pool"]))
            block.sync(mk(self.engs["sp"]))

NCORES = 8
S_ALL = 16384
NT = S_ALL // NCORES
D = 1024
W = 1024
NTX = NT + W
NCH = NT // 64
NWCH = W // 64
NE = 32
CAP = 384
ALPHA = 2.0 ** 0.25
BF16 = mybir.dt.bfloat16


def build_program(debug=False, stop=None, nhead=4, nseq=None):
    nc = bass.Bass("TRN2", target_bir_lowering=False)

    def din(name, shape, dt=F32):
        return nc.dram_tensor(name, list(shape), dt, kind="ExternalInput").ap()

    x_d = din("x", [NT, D]); xw_d = din("xw", [W, D]); mem_d = din("mem", [256, D])
    w_in_d = din("w_in", [D, 2560]); lbl_d = din("lb_logits", [2, 512]); hnw_d = din("hgrn_norm_w", [1, 512])
    wpool_d = din("w_pool", [4, 128, 128]); pscale_d = din("pool_scale", [4, 128]); w_out_d = din("w_out", [D, D])
    lnw_d = [din(f"ln{i}_w", [1, D]) for i in (1, 2, 3)]
    lnb_d = [din(f"ln{i}_b", [1, D]) for i in (1, 2, 3)]
    wq_d = din("w_xq", [D, D]); wk_d = din("w_xk", [D, D]); wv_d = din("w_xv", [D, D]); wo_d = din("w_xo", [D, D])
    wr_d = din("w_router", [D, NE]); br_d = din("b_router", [1, NE])
    NEW = NE if stop is None else 1
    w1_d = din("w1", [NEW, D, 2 * D]); b1_d = din("b1", [NE, 2 * D]); w2_d = din("w2", [NEW, D, D]); b2_d = din("b2", [NE, D])
    ident_d = din("c_ident", [128, 128]); m12_d = din("c_m12", [64, 128]); m3_d = din("c_m3", [64, 64])
    ustr_d = din("c_ustr", [128, 128]); ones_d = din("c_ones", [128, 128])
    iota_d = din("c_iota", [128, NE]); base_d = din("c_base", [128, NE]); inv16_d = din("c_inv16", [128, 64])
    out_d = nc.dram_tensor("out", [NT, D], F32, kind="ExternalOutput").ap()
    kind_dbg = "ExternalOutput" if debug else "Internal"
    x1_d = nc.dram_tensor("x1s", [NT, D], F32, kind=kind_dbg).ap()
    x2_d = nc.dram_tensor("x2s", [NT, D], F32, kind=kind_dbg).ap()
    mix_d = nc.dram_tensor("mixs", [8, 128, NT], BF16).ap()
    NCA = (W + NT) // 64
    hs_v = nc.dram_tensor("hs_v", [4, 64, NCA * 128], BF16).ap()
    hs_k = nc.dram_tensor("hs_k", [4, 64, NCA * 128], BF16).ap()
    hs_q = nc.dram_tensor("hs_q", [4, 128, NT], BF16).ap()
    hs_s = nc.dram_tensor("hs_s", [4, 64, (NT // 64) * 64], BF16).ap()
    hs_g = nc.dram_tensor("hs_g", [4, 64, (NT // 64) * 128], BF16).ap()
    hs_e = nc.dram_tensor("hs_e", [4, 128, NCA], F32).ap()
    xbuf_d = nc.dram_tensor("xbuf", [NE * CAP + 128, D], BF16).ap()
    ybuf_d = nc.dram_tensor("ybuf", [NE * CAP + 128, D], F32).ap()

    with ExitStack() as st:
        P = Prog(nc, st)
        PS = [st.enter_context(nc.psum_tensor(f"psb{i}", [128, 512], F32)) for i in range(8)]
        bPS = [P.buf(f"ps{i}") for i in range(8)]
        for b_ in bPS:
            b_.excl = True

        def mm(out, lhsT, rhs, start, stop, reads, wbuf):
            P.op("pe", lambda e: e.matmul(out, lhsT=lhsT, rhs=rhs, start=start, stop=stop), reads, [wbuf])

        def tr(out, in_, ident, reads, wbuf):
            P.op("pe", lambda e: e.transpose(out, in_, ident), reads, [wbuf])

        def ld(out, in_, wbuf, reads=(), q="sp", chain=True):
            P.dma(q, lambda e: e.dma_start(out=out, in_=in_), list(reads), [wbuf], chain=chain)

        def dve(fn, reads, writes):
            P.op("dve", fn, reads, writes)

        def act(fn, reads, writes):
            P.op("act", fn, reads, writes)

        ident = P.sb([128, 128]); bconst = P.buf("const")
        m12 = P.sb([64, 128]); m3 = P.sb([64, 64]); ustr = P.sb([128, 128]); ones = P.sb([128, 128])
        iota_e = P.sb([128, NE]); base_e = P.sb([128, NE]); inv16 = P.sb([128, 64])
        for t_, d_ in ((ident, ident_d), (m12, m12_d), (m3, m3_d), (ustr, ustr_d), (ones, ones_d),
                       (iota_e, iota_d), (base_e, base_d), (inv16, inv16_d)):
            ld(t_, d_, bconst, chain=False)
        brow = P.sb([128, NE]); ld(brow, br_d.broadcast_to([128, NE]), bconst, chain=False)
        lbrow = P.sb([64, 512]); omlrow = P.sb([64, 512]); hnwrow = P.sb([64, 512]); tmprow = P.sb([64, 512])
        blb = P.buf("lb")
        ld(lbrow, lbl_d[0:1, :].broadcast_to([64, 512]), blb, chain=False)
        ld(tmprow, lbl_d[1:2, :].broadcast_to([64, 512]), blb, chain=False)
        ld(hnwrow, hnw_d.broadcast_to([64, 512]), blb, chain=False)
        dve(lambda e: e.tensor_sub(out=lbrow, in0=lbrow, in1=tmprow), [blb], [blb])
        act(lambda e: e.activation(out=lbrow, in_=lbrow, func=AF.Sigmoid), [blb], [blb])
        dve(lambda e: e.tensor_scalar(out=omlrow, in0=lbrow, scalar1=-1.0, scalar2=1.0, op0=ALU.mult, op1=ALU.add), [blb], [blb])
        lb8 = P.sb([8, 128]); lbc = P.sb([128, 8]); omlc = P.sb([128, 4]); pscr = P.sb([4, 128]); pscc = P.sb([128, 4])
        ld(lb8, lbl_d.rearrange("a (h k) -> (a h) k", k=128), blb)
        ld(pscr, pscale_d, blb)
        tr(PS[0][:, 0:8], lb8, ident[0:8, 0:8], [blb, bconst], bPS[0])
        tr(PS[0][:, 8:12], pscr, ident[0:4, 0:4], [blb, bconst], bPS[0])
        dve(lambda e: e.tensor_copy(out=lbc, in_=PS[0][:, 0:8]), [bPS[0]], [blb])
        dve(lambda e: e.tensor_sub(out=lbc[:, 0:4], in0=lbc[:, 0:4], in1=lbc[:, 4:8]), [blb], [blb])
        dve(lambda e: e.tensor_copy(out=pscc, in_=PS[0][:, 8:12]), [bPS[0]], [blb])
        act(lambda e: e.activation(out=lbc[:, 0:4], in_=lbc[:, 0:4], func=AF.Sigmoid), [blb], [blb])
        dve(lambda e: e.tensor_scalar(out=omlc, in0=lbc[:, 0:4], scalar1=-1.0, scalar2=1.0, op0=ALU.mult, op1=ALU.add), [blb], [blb])
        slots_all = P.sb([128, 64], I32); gates_all = P.sb([128, 16, 4]); mask_all = P.sb([128, 16, NE])
        broute = P.buf("route")
        xin = [P.sb([128, D]) for _ in range(2)]; bxin = [P.buf(f"xin{i}") for i in range(2)]
        lnst = [(P.sb([128, 2, 6]), P.sb([128, 2]), P.sb([128, 1]), P.buf(f"lnst{i}")) for i in range(2)]
        P.mark()
        xT = P.sb([128, 8, NTX], BF16); bxT = P.buf("xT")

        def run_lockstep(gens, width=2):
            it = iter(gens)
            active = []
            for _ in range(width):
                g = next(it, None)
                if g is not None:
                    active.append(g)
            while active:
                for g in list(active):
                    try:
                        next(g)
                    except StopIteration:
                        active.remove(g)
                        n = next(it, None)
                        if n is not None:
                            active.append(n)

        for ti in range(NTX // 128):
            s_ = ti % 2
            src = xw_d[ti * 128:(ti + 1) * 128, :] if ti < W // 128 else x_d[(ti - W // 128) * 128:(ti - W // 128 + 1) * 128, :]
            ld(xin[s_], src, bxin[s_])
            for half in range(2):
                pi = (ti * 2 + half) % 4
                for j in range(4):
                    kc = half * 4 + j
                    tr(PS[pi][:, j * 128:(j + 1) * 128], xin[s_][:, kc * 128:(kc + 1) * 128], ident, [bxin[s_], bconst], bPS[pi])
                o = xT[:, half * 4:half * 4 + 4, ti * 128:(ti + 1) * 128]
                if half == 0:
                    dve(lambda e, o=o, pi=pi: e.tensor_copy(out=o, in_=PS[pi][:, :].rearrange("p (a b) -> p a b", a=4)), [bPS[pi]], [bxT])
                else:
                    act(lambda e, o=o, pi=pi: e.copy(out=o, in_=PS[pi][:, :].rearrange("p (a b) -> p a b", a=4)), [bPS[pi]], [bxT])

        w_in_v = w_in_d.rearrange("(c p) n -> p c n", p=128)

        bxb = P.buf("xbuf")
        ztile = P.sb([128, D], BF16); bzt = P.buf("ztile")
        P.op("pool", lambda e: e.memset(ztile, 0.0), [], [bzt])

        byb = P.buf("ybuf")
        ld(ybuf_d[NE * CAP:NE * CAP + 128, :], ztile, byb, reads=[bzt], q="pool")
        ld(xbuf_d[NE * CAP:NE * CAP + 128, :], ztile, bxb, reads=[bzt], q="sp")

        def zero_fill(e0, e1):
            for e_ in range(e0, e1):
                for a in range(3):
                    ld(xbuf_d[e_ * CAP + a * 128:e_ * CAP + (a + 1) * 128, :], ztile, bxb, reads=[bzt], q="sp")

        P.mark()
        NC_ALL = NWCH + NCH
        rmask = P.sb([128, NTX], BF16); brm = P.buf("rmask")
        P.op("pool", lambda e: e.memset(rmask, 1.0), [], [brm])
        dve(lambda e: e.memset(rmask.rearrange("p (c t) -> p c t", t=64)[:, :, 0:1], 0.0), [brm], [brm])
        identb = P.sb([128, 128], BF16)
        act(lambda e: e.copy(out=identb, in_=ident), [bconst], [brm])
        nomlc = P.sb([128, 4])
        dve(lambda e: e.tensor_scalar(out=nomlc, in0=omlc, scalar1=-1.0, scalar2=None, op0=ALU.mult), [blb], [brm])
        wfms = [P.sb([128, 8, 256], BF16) for _ in range(2)]; bwfms = [P.buf(f"wfm{i}") for i in range(2)]
        wtks = [P.sb([128, 8, 256], BF16) for _ in range(2)]; bwtks = [P.buf(f"wtk{i}") for i in range(2)]
        q_fm = P.sb([128, NT]); bqf = P.buf("qfm")
        zb = P.sb([128, NTX]); bzb = P.buf("zb")
        keyf = P.sb([128, NTX]); bkf = P.buf("keyf")
        bcum = P.sb([128, NTX]); bbc = P.buf("bcum")
        scr2 = P.sb([128, NT]); bscr2 = P.buf("scr2")
        q_inter = P.sb([128, NT], BF16); bqn = P.buf("qinter")
        qki = P.sb([128, 2 * NT], BF16); bqki = P.buf("qki")
        q_intra = qki[:, 0:NT]; k_intra = qki[:, NT:2 * NT]; kst_fm = qki[:, 0:NTX]
        v_tok = P.sb([64, NC_ALL, 128], BF16); k_state = P.sb([64, NC_ALL, 128], BF16); bvt = P.buf("vt"); bks = P.buf("ks")
        g_tok = P.sb([64, NCH, 128], BF16); bg = P.buf("g")
        eb_last = P.sb([128, NC_ALL]); beb = P.buf("eb")
        scT = P.sb([64, NCH, 64], BF16); bsc = P.buf("scT")
        S32 = [P.sb([128, 128]) for _ in range(2)]; Sbf = [P.sb([128, 128], BF16) for _ in range(2)]
        bS = [P.buf(f"S{i}") for i in range(2)]; bSb = [P.buf(f"Sbf{i}") for i in range(2)]
        bds = [P.buf(f"ds{i}") for i in range(8)]
        for b_ in bds:
            b_.excl = True
        o_sbs = [scr2[0:64, 0:1024].rearrange("p (a b) -> p a b", a=8), scr2[0:64, 1024:2048].rearrange("p (a b) -> p a b", a=8)]; bos = [P.buf(f"o{i}") for i in range(2)]; tsq = P.sb([64, 8, 128]); btsq = P.buf("tsq")
        ssq = P.sb([64, 8]); rstd = P.sb([64, 8]); bss = P.buf("ss")
        stg = [P.sb([128, 512], BF16) for _ in range(2)]; bstg = [P.buf(f"stg{i}") for i in range(2)]
        bmixd = P.buf("mixd")
        mask64 = m12[:, 64:128]
        zb3 = zb.rearrange("p (c t) -> p c t", t=64); bc3 = bcum.rearrange("p (c t) -> p c t", t=64)
        nstg = 0

        def load_head_w(hh):
            wf = wfms[hh % 2]; bwf = bwfms[hh % 2]; wt = wtks[hh % 2]; bwt = bwtks[hh % 2]
            ld(wf[:, :, 0:128], w_in_v[:, :, hh * 128:(hh + 1) * 128], bwf, q="pool")
            ld(wf[:, :, 128:256], w_in_v[:, :, 512 + hh * 128:512 + (hh + 1) * 128], bwf, q="pool")
            ld(wt[:, :, 0:128], w_in_v[:, :, 1024 + hh * 128:1024 + (hh + 1) * 128], bwt, q="pool")
            ld(wt[:, :, 128:256], w_in_v[:, :, 1536 + hh * 128:1536 + (hh + 1) * 128], bwt, q="pool")

        bhs = [P.buf(f"hs{i}") for i in range(4)]
        bzbh = [P.buf(f"zbh{i}") for i in range(2)]; bbch = [P.buf(f"bch{i}") for i in range(2)]; bkfh = [P.buf(f"kfh{i}") for i in range(2)]
        bscr2h = [P.buf(f"s2h{i}") for i in range(2)]; bqkih = [P.buf(f"qkh{i}") for i in range(2)]; bkst = [P.buf(f"kst{i}") for i in range(2)]
        bebh = [P.buf(f"ebh{i}") for i in range(2)]; bqnh = [P.buf(f"qnh{i}") for i in range(2)]; bsch = [P.buf(f"sch{i}") for i in range(2)]
        for h in range(nhead):
            wfm = wfms[h % 2]; bwfm = bwfms[h % 2]; wtk = wtks[h % 2]; bwtk = bwtks[h % 2]
            if h == 0:
                load_head_w(0)
            zero_fill(h * 8, h * 8 + 8)
            for tb in range(4):
                pi = tb % 2
                for kc in range(8):
                    mm(PS[pi][:, :], wfm[:, kc, 0:128], xT[:, kc, W + tb * 512:W + (tb + 1) * 512], kc == 0, kc == 7, [bwfm, bxT], bPS[pi])
                dve(lambda e, pi=pi, tb=tb: e.tensor_copy(out=q_fm[:, tb * 512:(tb + 1) * 512], in_=PS[pi][:, :]), [bPS[pi]], [bqf])
            for tb in range(NTX // 512):
                pi = 2 + tb % 2
                for kc in range(8):
                    mm(PS[pi][:, :], wfm[:, kc, 128:256], xT[:, kc, tb * 512:(tb + 1) * 512], kc == 0, kc == 7, [bwfm, bxT], bPS[pi])
                act(lambda e, pi=pi, tb=tb: e.activation(out=zb[:, tb * 512:(tb + 1) * 512], in_=PS[pi][:, :], func=AF.Exp, scale=-1.0), [bPS[pi]], [bzb, bzbh[0], bzbh[1]])
            if stop == "C1":
                P.barrier(); P.emit(); return nc
            for c2 in range(NC_ALL // 2):
                pi = 4 + c2 % 4
                own2 = c2 * 2 - NWCH
                ncol = 256 if own2 >= 0 else 128
                for j in range(2):
                    c = c2 * 2 + j
                    for kc in range(8):
                        mm(PS[pi][0:64, j * 256:j * 256 + ncol], xT[:, kc, c * 64:(c + 1) * 64], wtk[:, kc, 0:ncol], kc == 0, kc == 7, [bwtk, bxT], bPS[pi])
                pv = PS[pi][0:64, :].rearrange("p (a b) -> p a b", a=2)
                dve(lambda e, pv=pv, c2=c2: e.tensor_copy(out=v_tok[:, c2 * 2:c2 * 2 + 2, :], in_=pv[:, :, 0:128]), [bPS[pi]], [bvt])
                if own2 >= 0:
                    act(lambda e, pv=pv, own2=own2: e.activation(out=g_tok[:, own2:own2 + 2, :], in_=pv[:, :, 128:256], func=AF.Silu), [bPS[pi]], [bg])
            if stop == "C2":
                P.barrier(); P.emit(); return nc
            P.op("pool", lambda e, h=h: e.tensor_mul(out=g_tok, in0=g_tok, in1=hnwrow[:, h * 128:(h + 1) * 128].unsqueeze(1).broadcast_to([64, NCH, 128])), [bg, blb], [bg])
            def chain_half(hf, h=h):
                HT = NTX // 2
                t0 = hf * HT; t1 = t0 + HT
                c0 = t0 // 64; c1 = t1 // 64
                o0 = max(t0, W) - W; o1 = t1 - W
                oc0 = o0 // 64; oc1 = o1 // 64
                ts_ = slice(t0, t1); tow = slice(W + o0, W + o1); to = slice(o0, o1)
                zbh = bzbh[hf]; bch = bbch[hf]; kfh = bkfh[hf]; s2h = bscr2h[hf]; qkh = bqkih[hf]
                act(lambda e: e.activation(out=zb[:, ts_], in_=zb[:, ts_], func=AF.Ln, bias=1.0, scale=1.0), [bzb, zbh], [zbh])
                yield
                act(lambda e: e.activation(out=zb[:, ts_], in_=zb[:, ts_], func=AF.Exp, scale=-1.0), [zbh], [zbh])
                yield
                dve(lambda e: e.tensor_scalar(out=bcum[:, ts_], in0=zb[:, ts_], scalar1=omlc[:, h:h + 1], scalar2=lbc[:, h:h + 1], op0=ALU.mult, op1=ALU.add), [zbh, blb], [bch])
                dve(lambda e: e.tensor_scalar(out=keyf[:, ts_], in0=zb[:, ts_], scalar1=nomlc[:, h:h + 1], scalar2=omlc[:, h:h + 1], op0=ALU.mult, op1=ALU.add), [zbh, blb, brm], [kfh])
                yield
                act(lambda e: e.activation(out=bcum[:, ts_], in_=bcum[:, ts_], func=AF.Ln), [bch], [bch])
                yield
                dve(lambda e: e.tensor_tensor_scan(out=zb[:, ts_], data0=rmask[:, ts_], data1=bcum[:, ts_], initial=0.0, op0=ALU.mult, op1=ALU.add), [brm, bch, zbh], [zbh])
                yield
                act(lambda e: e.activation(out=bcum[:, ts_], in_=zb[:, ts_], func=AF.Exp), [zbh, bch], [bch])
                yield
                dve(lambda e: e.tensor_copy(out=eb_last[:, c0:c1], in_=bc3[:, c0:c1, 63]), [bch], [bebh[hf]])
                dve(lambda e: e.tensor_mul(out=q_inter[:, to], in0=q_fm[:, to], in1=bcum[:, tow]), [bch, bqf], [bqnh[hf]])
                dve(lambda e: e.tensor_sub(out=bc3[:, NWCH + oc0:NWCH + oc1, :], in0=zb3[:, NWCH + oc0:NWCH + oc1, :],
                                            in1=zb3[:, NWCH + oc0:NWCH + oc1, 31:32].broadcast_to([128, oc1 - oc0, 64])), [zbh, bch, bebh[hf], bqnh[hf]], [bch])
                yield
                act(lambda e: e.activation(out=scr2[:, to], in_=bcum[:, tow], func=AF.Exp), [bch], [s2h])
                yield
                dve(lambda e: e.tensor_mul(out=q_intra[:, to], in0=q_fm[:, to], in1=scr2[:, to]), [s2h, bqf], [qkh])
                act(lambda e: e.activation(out=scr2[:, to], in_=bcum[:, tow], func=AF.Exp, scale=-1.0), [bch, s2h], [s2h])
                yield
                dve(lambda e: e.tensor_mul(out=k_intra[:, to], in0=keyf[:, tow], in1=scr2[:, to]), [s2h, kfh], [qkh])
                yield
                for g8 in range(oc0 // 8, oc1 // 8):
                    pi = hf
                    for cj in range(8):
                        c = g8 * 8 + cj
                        mm(PS[pi][0:64, cj * 64:(cj + 1) * 64], k_intra[:, c * 64:(c + 1) * 64], q_intra[:, c * 64:(c + 1) * 64], True, True, [qkh], bPS[pi])
                    yield
                    dve(lambda e, pi=pi, g8=g8: e.tensor_mul(out=scT[:, g8 * 8:(g8 + 1) * 8, :], in0=PS[pi][0:64, :].rearrange("p (a b) -> p a b", a=8),
                                                          in1=mask64.unsqueeze(1).broadcast_to([64, 8, 64])), [bPS[pi], bconst], [bsch[hf]])
                dve(lambda e: e.tensor_sub(out=bc3[:, c0:c1, :], in0=zb3[:, c0:c1, 63:64].broadcast_to([128, c1 - c0, 64]), in1=zb3[:, c0:c1, :]), [zbh, bch], [bch])
                yield
                act(lambda e: e.activation(out=bcum[:, ts_], in_=bcum[:, ts_], func=AF.Exp), [bch], [bch])
                yield

            run_lockstep([chain_half(0), chain_half(1)])
            for hf in range(2):
                HT = NTX // 2
                ts_ = slice(hf * HT, (hf + 1) * HT)
                dve(lambda e, ts_=ts_: e.tensor_mul(out=kst_fm[:, ts_], in0=keyf[:, ts_], in1=bcum[:, ts_]), [bbch[hf], bkfh[hf], bqkih[0], bqkih[1]], [bkst[hf], bqkih[0], bqkih[1]])
            for g4 in range(NC_ALL // 4):
                pi = 2 + g4 % 2
                hf = 0 if g4 * 4 < NC_ALL // 2 else 1
                for cj in range(4):
                    c = g4 * 4 + cj
                    mm(PS[pi][0:64, cj * 128:(cj + 1) * 128], kst_fm[:, c * 64:(c + 1) * 64], identb, True, True, [bkst[hf], brm], bPS[pi])
                o_ = k_state[:, g4 * 4:g4 * 4 + 4, :]
                if g4 % 2 == 0:
                    act(lambda e, pi=pi, o_=o_: e.copy(out=o_, in_=PS[pi][0:64, :].rearrange("p (a b) -> p a b", a=4)), [bPS[pi]], [bks])
                else:
                    dve(lambda e, pi=pi, o_=o_: e.tensor_copy(out=o_, in_=PS[pi][0:64, :].rearrange("p (a b) -> p a b", a=4)), [bPS[pi]], [bks])
            if stop == "C6":
                P.barrier(); P.emit(); return nc
            if h + 1 < nhead:
                load_head_w(h + 1)
            ld(hs_v[h], v_tok.rearrange("p a b -> p (a b)"), bhs[h], reads=[bvt])
            ld(hs_k[h], k_state.rearrange("p a b -> p (a b)"), bhs[h], reads=[bks])
            ld(hs_q[h], q_inter, bhs[h], reads=bqnh)
            ld(hs_s[h], scT.rearrange("p a b -> p (a b)"), bhs[h], reads=bsch)
            ld(hs_g[h], g_tok.rearrange("p a b -> p (a b)"), bhs[h], reads=[bg])
            ld(hs_e[h], eb_last, bhs[h], reads=bebh)
        P.release()
        P.barrier()

        if stop == "C":
            P.emit()
            return nc
        P.mark()
        NC_ALL = NWCH + NCH
        R_v = [P.sb([64, NC_ALL, 128], BF16) for _ in range(2)]; R_k = [P.sb([64, NC_ALL, 128], BF16) for _ in range(2)]
        R_q = [P.sb([128, NT], BF16) for _ in range(2)]; R_s = [P.sb([64, NCH, 64], BF16) for _ in range(2)]
        R_g = [P.sb([64, NCH, 128], BF16) for _ in range(2)]; R_e = [P.sb([128, NC_ALL]) for _ in range(2)]
        bR = [P.buf(f"R{i}") for i in range(2)]

        def rec_load(h, st):
            ld(R_v[st].rearrange("p a b -> p (a b)"), hs_v[h], bR[st], reads=[bhs[h]])
            ld(R_k[st].rearrange("p a b -> p (a b)"), hs_k[h], bR[st], reads=[bhs[h]])
            ld(R_q[st], hs_q[h], bR[st], reads=[bhs[h]])
            ld(R_s[st].rearrange("p a b -> p (a b)"), hs_s[h], bR[st], reads=[bhs[h]])
            ld(R_g[st].rearrange("p a b -> p (a b)"), hs_g[h], bR[st], reads=[bhs[h]])
            ld(R_e[st], hs_e[h], bR[st], reads=[bhs[h]])

        if nhead == 4:
            rec_load(0, 0)
            rec_load(1, 1)
        P.mark()
        NP = NT + 16
        wp = [P.sb([128, 8, 128], BF16) for _ in range(2)]; bwp = [P.buf(f"wp{i}") for i in range(2)]
        wpl = [P.sb([128, 128], BF16) for _ in range(2)]; bwpl = [P.buf(f"wpl{i}") for i in range(2)]
        p_sbs = [P.sb([128, NP])] * 2; sas = [P.sb([128, NP])] * 2; sbbs = [P.sb([128, NP])] * 2
        bps_ = [P.buf("p0")] * 2; bsas = [P.buf("sa0")] * 2; bsbs = [P.buf("sb0")] * 2
        d_bfs = [P.sb([128, NT], BF16)] * 2; bds_ = [P.buf("d0")] * 2
        t16s = [P.sb([128, 16])] * 2; bt16s = [P.buf("t160")] * 2
        stg2 = [P.sb([128, 512], BF16) for _ in range(4)]; bstg2 = [P.buf(f"stgp{i}") for i in range(4)]

        def group_D(gi, w):
            s_ = gi % 2
            B0 = 4 * s_
            p_sb = p_sbs[s_]; sa = sas[s_]; sbb = sbbs[s_]; bp = bps_[s_]; bsa = bsas[s_]; bsb2 = bsbs[s_]
            d_bf = d_bfs[s_]; bd = bds_[s_]; t16 = t16s[s_]; bt16 = bt16s[s_]
            ld(wp[s_], w_in_v[:, :, 2048 + gi * 128:2048 + (gi + 1) * 128], bwp[s_], q="pool")
            ld(wpl[s_], wpool_d[gi], bwpl[s_], q="pool")
            for tb in range(5):
                pi = B0 + tb % 4
                c0 = W - 16 + tb * 512; n = 512 if tb < 4 else 16
                for kc in range(8):
                    mm(PS[pi][:, 0:n], wp[s_][:, kc, :], xT[:, kc, c0:c0 + n], kc == 0, kc == 7, [bwp[s_], bxT], bPS[pi])
                yield
                act(lambda e, pi=pi, tb=tb, n=n: e.copy(out=p_sb[:, tb * 512:tb * 512 + n], in_=PS[pi][:, 0:n]), [bPS[pi]], [bp])
            cur, bcur = p_sb, bp
            step = 1
            bufs2 = [(sa, bsa), (sbb, bsb2)]
            k = 0
            while step < w:
                nxt, bnxt = bufs2[k % 2]
                yield
                dve(lambda e, cur=cur, nxt=nxt, step=step: e.tensor_add(out=nxt[:, step:NP], in0=cur[:, step:NP], in1=cur[:, 0:NP - step]), [bcur], [bnxt])
                cur, bcur = nxt, bnxt
                step *= 2; k += 1
            yield
            dve(lambda e, cur=cur, w=w: e.scalar_tensor_tensor(out=d_bf, in0=cur[:, 16:NP], scalar=1.0 / w, in1=p_sb[:, 16:NP], op0=ALU.mult, op1=ALU.subtract), [bcur, bp], [bd])
            dve(lambda e, cur=cur, gi=gi: e.tensor_mul(out=t16, in0=cur[:, 16:32], in1=inv16[:, gi * 16:(gi + 1) * 16]), [bcur, bconst], [bt16])
            dve(lambda e: e.tensor_sub(out=d_bf[:, 0:16], in0=t16, in1=p_sb[:, 16:32]), [bt16, bp, bd], [bd])
            for tb in range(4):
                pi = B0 + tb % 4
                mm(PS[pi][:, :], wpl[s_], d_bf[:, tb * 512:(tb + 1) * 512], True, True, [bwpl[s_], bd], bPS[pi])
                sg_ = 2 * s_ + tb % 2
                yield
                dve(lambda e, pi=pi, sg_=sg_, gi=gi: e.tensor_scalar(out=stg2[sg_], in0=PS[pi][:, :], scalar1=pscc[:, gi:gi + 1], scalar2=None, op0=ALU.mult),
                    [bPS[pi], blb], [bstg2[sg_]])
                ld(mix_d[4 + gi, :, tb * 512:(tb + 1) * 512], stg2[sg_], bmixds[4 + gi], reads=[bstg2[sg_]])
            yield

        bmixds = [None] * 4 + [P.buf("mixdp")] * 4
        run_lockstep([group_D(gi, w) for gi, w in enumerate((2, 4, 8, 16))], width=1)
        P.release()
        P.barrier()

        P.mark()
        R_S32 = [[P.sb([128, 128]) for _ in range(2)] for _ in range(2)]; R_Sbf = [[P.sb([128, 128], BF16) for _ in range(2)] for _ in range(2)]
        bRS = [[P.buf(f"RS{i}{j}") for j in range(2)] for i in range(2)]; bRSb = [[P.buf(f"RSb{i}{j}") for j in range(2)] for i in range(2)]
        R_o = [[P.sb([64, 8, 128]) for _ in range(2)] for _ in range(2)]; bRo = [[P.buf(f"Ro{i}{j}") for j in range(2)] for i in range(2)]
        R_tsq = [P.sb([64, 8, 128]) for _ in range(2)]; R_ssq = [P.sb([64, 8]) for _ in range(2)]; R_rstd = [P.sb([64, 8]) for _ in range(2)]
        bRss = [P.buf(f"Rss{i}") for i in range(2)]; bRtsq = [P.buf(f"Rtsq{i}") for i in range(2)]
        R_stg = [[P.sb([128, 512], BF16) for _ in range(2)] for _ in range(2)]; bRstg = [[P.buf(f"Rstg{i}{j}") for j in range(2)] for i in range(2)]
        bmixh = [P.buf(f"mixh{i}") for i in range(4)]

        def rec_head(h, st):
            B0 = 4 * st
            v_tok = R_v[st]; k_state = R_k[st]; q_inter = R_q[st]; scT = R_s[st]; g_tok = R_g[st]; eb_last = R_e[st]
            S32 = R_S32[st]; Sbf = R_Sbf[st]; bS = bRS[st]; bSb = bRSb[st]
            tsq = R_tsq[st]; ssq = R_ssq[st]; rstd = R_rstd[st]; bss = bRss[st]; btsq = bRtsq[st]
            if h >= 2:
                rec_load(h, st)
            dve(lambda e: e.memset(S32[0], 0.0), [], [bS[0]])
            dve(lambda e: e.memset(Sbf[0], 0.0), [], [bSb[0]])
            yield

            def issue_ds_group(g):
                pd = B0 + 1 + g % 2
                for c in range(g * 4, g * 4 + 4):
                    mm(PS[pd][:, (c % 4) * 128:(c % 4 + 1) * 128], k_state[:, c, :], v_tok[:, c, :], True, True, [bR[st]], bPS[pd])

            issue_ds_group(0)
            nstg = 0
            for c in range(NC_ALL):
                own = c - NWCH
                if c % 4 == 0 and c // 4 + 1 < NC_ALL // 4:
                    issue_ds_group(c // 4 + 1)
                cur = c % 2; nxt = (c + 1) % 2
                if own >= 0:
                    o_sb = R_o[st][(own // 8) % 2]; bo = bRo[st][(own // 8) % 2]
                    po = B0
                    osl = PS[po][0:64, (own % 4) * 128:(own % 4 + 1) * 128]
                    mm(osl, q_inter[:, own * 64:(own + 1) * 64], Sbf[cur], True, False, [bR[st], bSb[cur]], bPS[po])
                    mm(osl, scT[:, own, :], v_tok[:, c, :], False, True, [bR[st]], bPS[po])
                    if own % 4 == 3:
                        g4 = (own // 4) % 2
                        act(lambda e, po=po, g4=g4, o_sb=o_sb: e.copy(out=o_sb[:, g4 * 4:g4 * 4 + 4, :], in_=PS[po][0:64, :].rearrange("p (a b) -> p a b", a=4)), [bPS[po]], [bo])
                pd = B0 + 1 + (c // 4) % 2
                dve(lambda e, c=c, pd=pd, cur=cur, nxt=nxt: e.scalar_tensor_tensor(out=S32[nxt], in0=S32[cur], scalar=eb_last[:, c:c + 1], in1=PS[pd][:, (c % 4) * 128:(c % 4 + 1) * 128], op0=ALU.mult, op1=ALU.add),
                    [bS[cur], bR[st], bPS[pd]], [bS[nxt]])
                if c >= NWCH - 1 and c < NC_ALL - 1:
                    act(lambda e, nxt=nxt: e.copy(out=Sbf[nxt], in_=S32[nxt]), [bS[nxt]], [bSb[nxt]])
                yield
                if own >= 0 and own % 8 == 7:
                    g8 = own // 8
                    gsl = slice(g8 * 8, g8 * 8 + 8)
                    dve(lambda e, o_sb=o_sb: e.tensor_mul(out=tsq, in0=o_sb, in1=o_sb), [bo], [btsq])
                    dve(lambda e: e.tensor_reduce(out=ssq, in_=tsq, axis=AX.X, op=ALU.add), [btsq], [bss])
                    act(lambda e: e.activation(out=ssq, in_=ssq, func=AF.Ln, bias=1e-6, scale=1.0 / 128.0), [bss], [bss])
                    act(lambda e: e.activation(out=rstd, in_=ssq, func=AF.Exp, scale=-0.5), [bss], [bss])
                    yield
                    dve(lambda e, o_sb=o_sb: e.tensor_mul(out=o_sb, in0=o_sb, in1=rstd.unsqueeze(2).broadcast_to([64, 8, 128])), [bo, bss], [bo])
                    P.op("pool", lambda e, gsl=gsl, o_sb=o_sb: e.tensor_mul(out=o_sb, in0=o_sb, in1=g_tok[:, gsl, :]), [bo, bR[st]], [bo])
                    yield
                    pi = B0 + 3
                    for cj in range(8):
                        tr(PS[pi][:, cj * 64:(cj + 1) * 64], o_sb[:, cj, :], ident[0:64, 0:64], [bo, bconst], bPS[pi])
                    sg_ = nstg % 2; nstg += 1
                    yield
                    act(lambda e, pi=pi, sg_=sg_: e.copy(out=R_stg[st][sg_], in_=PS[pi][:, :]), [bPS[pi]], [bRstg[st][sg_]])
                    ld(mix_d[h, :, g8 * 512:(g8 + 1) * 512], R_stg[st][sg_], bmixh[h], reads=[bRstg[st][sg_]])
            yield

        if nhead == 4:
            run_lockstep([rec_head(0, 0), rec_head(1, 1)])
            run_lockstep([rec_head(2, 0), rec_head(3, 1)])
        P.release()
        P.release()
        P.release()
        P.mark()
        P.barrier()
        if stop == "C2":
            P.emit()
            return nc

        def layernorm(r, br, lw, lb_, blw, out, bout, par=0, use_pool=False, norm_on_act=False):
            stats, mv, rs, bst = lnst[par]
            dve(lambda e: e.bn_stats(out=stats[:, 0, :], in_=r[:, 0:512]), [br], [bst])
            dve(lambda e: e.bn_stats(out=stats[:, 1, :], in_=r[:, 512:1024]), [br], [bst])
            dve(lambda e: e.bn_aggr(out=mv, in_=stats[:, :, :].rearrange("p a b -> p (a b)")), [bst], [bst])
            act(lambda e: e.activation(out=rs, in_=mv[:, 1:2], func=AF.Ln, bias=1e-5, scale=1.0), [bst], [bst])
            act(lambda e: e.activation(out=rs, in_=rs, func=AF.Exp, scale=-0.5), [bst], [bst])
            if norm_on_act:
                dve(lambda e: e.scalar_tensor_tensor(out=mv[:, 1:2], in0=mv[:, 0:1], scalar=-1.0, in1=rs, op0=ALU.mult, op1=ALU.mult), [bst], [bst])
                act(lambda e: e.activation(out=r, in_=r, func=AF.Identity, bias=mv[:, 1:2], scale=rs[:, 0:1]), [br, bst], [br])
            else:
                dve(lambda e: e.tensor_scalar(out=r, in0=r, scalar1=mv[:, 0:1], scalar2=rs[:, 0:1], op0=ALU.subtract, op1=ALU.mult), [br, bst], [br])
            eng_ = "pool" if use_pool else "dve"
            P.op(eng_, lambda e: e.tensor_mul(out=r, in0=r, in1=lw), [br, blw], [br])
            P.op(eng_, lambda e: e.tensor_add(out=out, in0=r, in1=lb_), [br, blw], [bout])

        def load_ln(i):
            lw = P.sb([128, D]); lb_ = P.sb([128, D]); blw = P.buf(f"ln{i}")
            ld(lw, lnw_d[i].broadcast_to([128, D]), blw)
            ld(lb_, lnb_d[i].broadcast_to([128, D]), blw)
            return lw, lb_, blw

        P.mark()
        ln1 = load_ln(0); ln2 = load_ln(1)
        mixT = P.sb([128, 8, NT], BF16); bmixT = P.buf("mixT")
        ld(mixT, mix_d.rearrange("c p t -> p c t"), bmixT, reads=bmixh + bmixds[4:5])
        wbig = [P.sb([128, 8, D], BF16) for _ in range(2)]; bwbig = [P.buf(f"wbig{i}") for i in range(2)]
        ld(wbig[0], w_out_d.rearrange("(c p) n -> p c n", p=128), bwbig[0], q="pool")
        x1T = P.sb([128, 8, NT], BF16)
        rt = [P.sb([128, D]) for _ in range(2)]; brt = [P.buf(f"rt{i}") for i in range(2)]
        xo = [P.sb([128, D]) for _ in range(2)]; bxo = [P.buf(f"xo{i}") for i in range(2)]
        bx1ds = [P.buf(f"x1d{i}") for i in range(16)]; bx2ds = [P.buf(f"x2d{i}") for i in range(16)]
        bx1Ts = [P.buf(f"x1T{i}") for i in range(16)]

        xinE = [xin[0], xin[1], P.sb([128, D])]; bxinE = [bxin[0], bxin[1], P.buf("xin2")]
        rtE = [rt[0], rt[1], P.sb([128, D])]; brtE = [brt[0], brt[1], P.buf("rt2")]
        xoE = [xo[0], xo[1], P.sb([128, D])]; bxoE = [bxo[0], bxo[1], P.buf("xo2")]
        lnst.append((P.sb([128, 2, 6]), P.sb([128, 2]), P.sb([128, 1]), P.buf("lnst2")))

        def tile_E(ti):
            s_ = ti % 3
            B0 = 2 * s_
            ld(xinE[s_], x_d[ti * 128:(ti + 1) * 128, :], bxinE[s_])
            for half in range(2):
                pi = B0 + half
                for kc in range(8):
                    mm(PS[pi][:, :], mixT[:, kc, ti * 128:(ti + 1) * 128], wbig[0][:, kc, half * 512:(half + 1) * 512], kc == 0, kc == 7, [bmixT, bwbig[0]], bPS[pi])
                yield
                dve(lambda e, s_=s_, pi=pi, half=half: e.scalar_tensor_tensor(out=rtE[s_][:, half * 512:(half + 1) * 512], in0=xinE[s_][:, half * 512:(half + 1) * 512], scalar=ALPHA,
                                                                         in1=PS[pi][:, :], op0=ALU.mult, op1=ALU.add), [bxinE[s_], bPS[pi]], [brtE[s_]])
            yield
            layernorm(rtE[s_], brtE[s_], ln1[0], ln1[1], ln1[2], xoE[s_], bxoE[s_], par=s_, use_pool=True, norm_on_act=True)
            yield
            ld(x1_d[ti * 128:(ti + 1) * 128, :], xoE[s_], bx1ds[ti], reads=[bxoE[s_]])
            for half in range(2):
                pi = B0 + half
                for j in range(4):
                    kc = half * 4 + j
                    tr(PS[pi][:, j * 128:(j + 1) * 128], xoE[s_][:, kc * 128:(kc + 1) * 128], ident, [bxoE[s_], bconst], bPS[pi])
                o = x1T[:, half * 4:half * 4 + 4, ti * 128:(ti + 1) * 128]
                yield
                act(lambda e, o=o, pi=pi: e.copy(out=o, in_=PS[pi][:, :].rearrange("p (a b) -> p a b", a=4)), [bPS[pi]], [bx1Ts[ti]])
            yield

        run_lockstep([tile_E(ti) for ti in range(16)], width=3)
        P.barrier()

        qT = mixT; bqT = bmixT
        ld(wbig[1], wk_d.rearrange("(c p) n -> p c n", p=128), bwbig[1], q="pool")
        kT = P.sb([128, 8, 256], BF16); bkT = P.buf("kT"); v_m = P.sb([128, 2, D], BF16); bvm = P.buf("vm")
        P.mark()
        memT = P.sb([128, 8, 256], BF16); bmemT = P.buf("memT")
        P.release()
        for mt in range(2):
            ld(xin[mt], mem_d[mt * 128:(mt + 1) * 128, :], bxin[mt])
            for half in range(2):
                pi = 4 + half
                for j in range(4):
                    kc = half * 4 + j
                    tr(PS[pi][:, j * 128:(j + 1) * 128], xin[mt][:, kc * 128:(kc + 1) * 128], ident, [bxin[mt], bconst], bPS[pi])
                o = memT[:, half * 4:half * 4 + 4, mt * 128:(mt + 1) * 128]
                dve(lambda e, o=o, pi=pi: e.tensor_copy(out=o, in_=PS[pi][:, :].rearrange("p (a b) -> p a b", a=4)), [bPS[pi]], [bmemT])
        for fc in range(8):
            pi = fc % 2
            for kc in range(8):
                mm(PS[pi][:, 0:256], wbig[1][:, kc, fc * 128:(fc + 1) * 128], memT[:, kc, :], kc == 0, kc == 7, [bwbig[1], bmemT], bPS[pi])
            act(lambda e, pi=pi, fc=fc: e.copy(out=kT[:, fc, :], in_=PS[pi][:, 0:256]), [bPS[pi]], [bkT])
        ld(wbig[0], wv_d.rearrange("(c p) n -> p c n", p=128), bwbig[0], q="pool")
        for mt in range(2):
            for half in range(2):
                pi = 2 + half
                for kc in range(8):
                    mm(PS[pi][:, :], memT[:, kc, mt * 128:(mt + 1) * 128], wbig[0][:, kc, half * 512:(half + 1) * 512], kc == 0, kc == 7, [bwbig[0], bmemT], bPS[pi])
                act(lambda e, pi=pi, mt=mt, half=half: e.copy(out=v_m[:, mt, half * 512:(half + 1) * 512], in_=PS[pi][:, :]), [bPS[pi]], [bvm])
        ld(wbig[1], wq_d.rearrange("(c p) n -> p c n", p=128), bwbig[1], q="pool")
        for fc in range(8):
            for tb in range(4):
                pi = (fc * 4 + tb) % 4
                for kc in range(8):
                    mm(PS[pi][:, :], wbig[1][:, kc, fc * 128:(fc + 1) * 128], x1T[:, kc, tb * 512:(tb + 1) * 512], kc == 0, kc == 7, [bwbig[1]] + bx1Ts[tb * 4:tb * 4 + 4], bPS[pi])
                if tb % 2 == 0:
                    act(lambda e, pi=pi, fc=fc, tb=tb: e.activation(out=qT[:, fc, tb * 512:(tb + 1) * 512], in_=PS[pi][:, :], func=AF.Copy, scale=1.0 / 16.0), [bPS[pi]], [bqT])
                else:
                    dve(lambda e, pi=pi, fc=fc, tb=tb: e.tensor_scalar(out=qT[:, fc, tb * 512:(tb + 1) * 512], in0=PS[pi][:, :], scalar1=1.0 / 16.0, scalar2=None, op0=ALU.mult), [bPS[pi]], [bqT])
        ld(wbig[0], wo_d.rearrange("(c p) n -> p c n", p=128), bwbig[0], q="pool")
        wr32 = P.sb([128, 8, NE]); bwr = P.buf("wr")
        P.dma("sp", lambda e: e.dma_start(out=wr32, in_=wr_d.rearrange("(c p) n -> p c n", p=128)), [], [bwr, bmemT])
        pexp = [P.sb([128, 4, 256]) for _ in range(2)]; bpe = [P.buf(f"pexp{i}") for i in range(2)]
        nmx = [P.sb([128, 12]) for _ in range(2)]; bnm = [P.buf(f"nmx{i}") for i in range(2)]
        pT = [P.sb([128, 8, 128], BF16) for _ in range(2)]; bpT = [P.buf(f"pT{i}") for i in range(2)]
        oT = [P.sb([128, 8, 128], BF16) for _ in range(2)]; boT = [P.buf(f"oT{i}") for i in range(2)]
        x2T = [P.sb([128, 8, 128]) for _ in range(2)]; bx2T = [P.buf(f"x2T{i}") for i in range(2)]
        lgs = [P.sb([128, NE]) for _ in range(2)]; top8s = [P.sb([128, 8]) for _ in range(2)]; idx8s = [P.sb([128, 8], U32) for _ in range(2)]
        idxfs = [P.sb([128, 4]) for _ in range(2)]; blgs = [P.buf(f"lg{i}") for i in range(2)]
        gexs = [P.sb([128, 6]) for _ in range(2)]; rksls = [P.sb([128, NE]) for _ in range(2)]; ohss = [P.sb([128, NE]) for _ in range(2)]
        slotfs = [P.sb([128, 4]) for _ in range(2)]; brks = [P.buf(f"rk{i}") for i in range(2)]
        bmask = [P.buf(f"mask{i}") for i in range(16)]; bslot = [P.buf(f"slot{i}") for i in range(16)]; bgate = [P.buf(f"gate{i}") for i in range(16)]

        bxbk = [P.buf(f"xbk{i}") for i in range(4)]
        xob = [P.sb([128, D], BF16) for _ in range(2)]; bxob = [P.buf(f"xob{i}") for i in range(2)]
        cum_all = P.sb([128, 16, NE]); bcumm = [P.buf(f"cum{i}") for i in range(16)]
        dve(lambda e: e.memset(cum_all[:, 0, :], 0.0), [], [bcumm[0]])

        def tile_F(ti):
            s_ = ti % 2; d_ = s_
            B0 = 4 * s_
            tsl = slice(ti * 128, (ti + 1) * 128)
            lg = lgs[s_]; top8 = top8s[s_]; idx8 = idx8s[s_]; idxf = idxfs[s_]; blg = blgs[s_]
            gex = gexs[s_]; rksl = rksls[s_]; ohs = ohss[s_]; slotf = slotfs[s_]; brk = brks[s_]
            ld(xin[s_], x1_d[tsl, :], bxin[s_], reads=[bx1ds[ti]])
            for hp in range(2):
                pb = B0 + hp
                for hh in range(2):
                    h = 2 * hp + hh
                    for j in range(2):
                        mm(PS[pb][:, hh * 256:(hh + 1) * 256], qT[:, 2 * h + j, tsl], kT[:, 2 * h + j, :], j == 0, j == 1, [bqT, bkT], bPS[pb])
                yield
                dve(lambda e, hp=hp, pb=pb, d_=d_: e.tensor_reduce(out=nmx[d_][:, 2 * hp:2 * hp + 2], in_=PS[pb][:, :].rearrange("p (a b) -> p a b", a=2), axis=AX.X, op=ALU.max, negate=True),
                    [bPS[pb]], [bnm[d_]])
                for hh in range(2):
                    h = 2 * hp + hh
                    act(lambda e, pb=pb, hh=hh, h=h, d_=d_: e.activation(out=pexp[d_][:, h, :], in_=PS[pb][:, hh * 256:(hh + 1) * 256], func=AF.Exp, bias=nmx[d_][:, h:h + 1], scale=1.0,
                                                                       accum_out=nmx[d_][:, 4 + h:5 + h]), [bPS[pb], bnm[d_]], [bpe[d_], bnm[d_]])
            yield
            dve(lambda e, d_=d_: e.reciprocal(out=nmx[d_][:, 8:12], in_=nmx[d_][:, 4:8]), [bnm[d_]], [bnm[d_]])
            dve(lambda e, d_=d_: e.tensor_mul(out=pexp[d_], in0=pexp[d_], in1=nmx[d_][:, 8:12].unsqueeze(2).broadcast_to([128, 4, 256])), [bpe[d_], bnm[d_]], [bpe[d_]])
            yield
            for hp in range(2):
                pj = B0 + 2 + hp
                for hh in range(2):
                    h = 2 * hp + hh
                    for mt in range(2):
                        tr(PS[pj][:, (hh * 2 + mt) * 128:(hh * 2 + mt + 1) * 128], pexp[d_][:, h, mt * 128:(mt + 1) * 128], ident, [bpe[d_], bconst], bPS[pj])
                o_ = pT[d_][:, hp * 4:hp * 4 + 4, :]
                yield
                if hp == 0:
                    act(lambda e, pj=pj, o_=o_: e.copy(out=o_, in_=PS[pj][:, :].rearrange("p (a b) -> p a b", a=4)), [bPS[pj]], [bpT[d_]])
                else:
                    dve(lambda e, pj=pj, o_=o_: e.tensor_copy(out=o_, in_=PS[pj][:, :].rearrange("p (a b) -> p a b", a=4)), [bPS[pj]], [bpT[d_]])
            yield
            for hp in range(2):
                po = B0 + hp
                for hh in range(2):
                    h = 2 * hp + hh
                    for j in range(2):
                        fc = 2 * h + j
                        for mt in range(2):
                            mm(PS[po][:, (hh * 2 + j) * 128:(hh * 2 + j + 1) * 128], v_m[:, mt, fc * 128:(fc + 1) * 128], pT[d_][:, h * 2 + mt, :], mt == 0, mt == 1, [bvm, bpT[d_]], bPS[po])
                o_ = oT[d_][:, hp * 4:hp * 4 + 4, :]
                yield
                if hp == 0:
                    dve(lambda e, po=po, o_=o_: e.tensor_copy(out=o_, in_=PS[po][:, :].rearrange("p (a b) -> p a b", a=4)), [bPS[po]], [boT[d_]])
                else:
                    act(lambda e, po=po, o_=o_: e.copy(out=o_, in_=PS[po][:, :].rearrange("p (a b) -> p a b", a=4)), [bPS[po]], [boT[d_]])
            yield
            for half in range(2):
                pi = B0 + 2 + half
                for fc in range(8):
                    mm(PS[pi][:, :], oT[d_][:, fc, :], wbig[0][:, fc, half * 512:(half + 1) * 512], fc == 0, fc == 7, [boT[d_], bwbig[0]], bPS[pi])
                yield
                dve(lambda e, s_=s_, pi=pi, half=half: e.scalar_tensor_tensor(out=rt[s_][:, half * 512:(half + 1) * 512], in0=xin[s_][:, half * 512:(half + 1) * 512], scalar=ALPHA,
                                                                         in1=PS[pi][:, :], op0=ALU.mult, op1=ALU.add), [bxin[s_], bPS[pi]], [brt[s_]])
            yield
            layernorm(rt[s_], brt[s_], ln2[0], ln2[1], ln2[2], xo[s_], bxo[s_], par=s_, norm_on_act=True)
            yield
            ld(x2_d[tsl, :], xo[s_], bx2ds[ti], reads=[bxo[s_]])
            act(lambda e, s_=s_: e.copy(out=xob[s_], in_=xo[s_]), [bxo[s_]], [bxob[s_]])
            for half in range(2):
                pi = B0 + half
                for j in range(4):
                    kc = half * 4 + j
                    tr(PS[pi][:, j * 128:(j + 1) * 128], xo[s_][:, kc * 128:(kc + 1) * 128], ident, [bxo[s_], bconst], bPS[pi])
                o = x2T[s_][:, half * 4:half * 4 + 4, :]
                yield
                act(lambda e, o=o, pi=pi: e.copy(out=o, in_=PS[pi][:, :].rearrange("p (a b) -> p a b", a=4)), [bPS[pi]], [bx2T[s_]])
            yield
            pl = B0 + 2; pr = B0 + 3
            for kc in range(8):
                mm(PS[pl][:, 0:NE], x2T[s_][:, kc, :], wr32[:, kc, :], kc == 0, kc == 7, [bx2T[s_], bwr], bPS[pl])
            yield
            dve(lambda e: e.tensor_add(out=lg, in0=PS[pl][:, 0:NE], in1=brow), [bPS[pl], bconst], [blg])
            dve(lambda e: e.max(out=top8, in_=lg), [blg], [blg])
            dve(lambda e: e.max_index(out=idx8, in_max=top8, in_values=lg), [blg], [blg])
            dve(lambda e: e.tensor_copy(out=idxf, in_=idx8[:, 0:4]), [blg], [blg])
            dve(lambda e, ti=ti: e.tensor_scalar(out=mask_all[:, ti, :], in0=lg, scalar1=top8[:, 3:4], scalar2=None, op0=ALU.is_ge), [blg], [bmask[ti]])
            yield
            dve(lambda e: e.tensor_scalar(out=gex[:, 4:5], in0=top8[:, 0:1], scalar1=-1.0, scalar2=None, op0=ALU.mult), [blg], [blg])
            act(lambda e: e.activation(out=gex[:, 0:4], in_=top8[:, 0:4], func=AF.Exp, bias=gex[:, 4:5], scale=1.0, accum_out=gex[:, 5:6]), [blg], [blg])
            yield
            dve(lambda e: e.reciprocal(out=gex[:, 5:6], in_=gex[:, 5:6]), [blg], [blg])
            dve(lambda e, ti=ti: e.tensor_scalar(out=gates_all[:, ti, :], in0=gex[:, 0:4], scalar1=gex[:, 5:6], scalar2=None, op0=ALU.mult), [blg], [bgate[ti]])
            if ti + 1 < 16:
                dve(lambda e, ti=ti: e.tensor_add(out=cum_all[:, ti + 1, :], in0=cum_all[:, ti, :], in1=mask_all[:, ti, :]), [bcumm[ti], bmask[ti]], [bcumm[ti + 1]])
            mm(PS[pr][:, 0:NE], ones, cum_all[:, ti, :], True, False, [bcumm[ti], bconst], bPS[pr])
            mm(PS[pr][:, 0:NE], ustr, mask_all[:, ti, :], False, True, [bmask[ti], bconst], bPS[pr])
            yield
            dve(lambda e: e.tensor_scalar(out=ohs, in0=PS[pr][:, 0:NE], scalar1=float(CAP), scalar2=1.0e6, op0=ALU.is_ge, op1=ALU.mult), [bPS[pr]], [brk])
            dve(lambda e: e.tensor_add(out=rksl, in0=PS[pr][:, 0:NE], in1=base_e), [bPS[pr], bconst], [brk])
            dve(lambda e: e.tensor_add(out=rksl, in0=rksl, in1=ohs), [brk], [brk])
            for k in range(4):
                dve(lambda e, k=k: e.scalar_tensor_tensor(out=ohs, in0=iota_e, scalar=idxf[:, k:k + 1], in1=rksl, op0=ALU.is_equal, op1=ALU.mult, accum_out=slotf[:, k:k + 1]),
                    [brk, blg, bconst], [brk])
            dve(lambda e: e.tensor_scalar(out=slotf, in0=slotf, scalar1=float(NE * CAP), scalar2=None, op0=ALU.min), [brk], [brk])
            dve(lambda e, ti=ti: e.tensor_copy(out=slots_all[:, ti * 4:ti * 4 + 4], in_=slotf), [brk], [bslot[ti]])
            yield
            for k in range(4):
                P.dma("pool", lambda e, ti=ti, k=k, s_=s_: e.indirect_dma_start(out=xbuf_d, out_offset=bass.IndirectOffsetOnAxis(ap=slots_all[:, ti * 4 + k:ti * 4 + k + 1], axis=0),
                                                                                 in_=xob[s_], in_offset=None),
                      [bxob[s_], bslot[ti], bxb], [bxbk[k]])
            yield

        run_lockstep([tile_F(ti) for ti in range(16)])
        P.release()
        P.release()
        P.barrier()

        if stop == "F":
            P.emit()
            return nc
        P.mark()
        b1r = P.sb([32, 2 * D]); bb1r = P.buf("b1r"); b1c = P.sb([128, 16, NE]); bb1c = P.buf("b1c")
        ld(b1r, b1_d, bb1r)
        for g4 in range(4):
            pi = g4 % 2
            for j in range(4):
                fc = g4 * 4 + j
                tr(PS[pi][:, j * NE:(j + 1) * NE], b1r[:, fc * 128:(fc + 1) * 128], ident[0:32, 0:32], [bb1r, bconst], bPS[pi])
            dve(lambda e, pi=pi, g4=g4: e.tensor_copy(out=b1c[:, g4 * 4:g4 * 4 + 4, :], in_=PS[pi][:, 0:4 * NE].rearrange("p (a b) -> p a b", a=4)), [bPS[pi]], [bb1c])
        xbT = P.sb([128, 8, CAP], BF16); bxbT = P.buf("xbT")
        w1e = [P.sb([128, 8, 2 * D], BF16) for _ in range(2)]; bw1e = [P.buf(f"w1e{i}") for i in range(2)]
        w2e = [P.sb([128, 8, D], BF16) for _ in range(2)]; bw2e = [P.buf(f"w2e{i}") for i in range(2)]
        xb2 = [P.sb([128, 3, D], BF16) for _ in range(2)]; bxb2 = [P.buf(f"xb2{i}") for i in range(2)]
        identb2 = P.sb([128, 128], BF16); bidb = P.buf("identb2")
        act(lambda e: e.copy(out=identb2, in_=ident), [bconst], [bidb])
        actT = P.sb([128, 8, CAP], BF16); bact = P.buf("actT")
        y_sb = [P.sb([128, 3, D])] * 2; by = [P.buf("y0")] * 2
        b2row = [P.sb([128, D]) for _ in range(2)]; bb2 = [P.buf(f"b2r{i}") for i in range(2)]
        g1 = [P.sb([128, CAP]) for _ in range(2)]; l0 = [P.sb([128, CAP]) for _ in range(2)]; sg = [P.sb([128, CAP]) for _ in range(2)]
        bg1 = [P.buf(f"g1{i}") for i in range(2)]; bl0 = [P.buf(f"l0{i}") for i in range(2)]; bsg = [P.buf(f"sg{i}") for i in range(2)]
        w1_v = w1_d.rearrange("e (c p) n -> e p c n", p=128)
        w2_v = w2_d.rearrange("e (c p) n -> e p c n", p=128)

        def load_w1(e_):
            ld(w1e[e_ % 2], w1_v[e_], bw1e[e_ % 2], q="pool")

        def load_w2(e_):
            ld(w2e[e_ % 2], w2_v[e_], bw2e[e_ % 2], q="pool")
            ld(b2row[e_ % 2], b2_d[e_:e_ + 1, :].broadcast_to([128, D]), bb2[e_ % 2])

        def load_x(e_):
            ld(xb2[e_ % 2], xbuf_d[e_ * CAP:(e_ + 1) * CAP, :].rearrange("(a p) d -> p a d", p=128), bxb2[e_ % 2], reads=[bxb] + bxbk)

        load_x(0)
        load_w1(0)
        load_w2(0)
        load_x(1)
        load_w1(1)
        load_w2(1)
        for e_ in range(NE):
            s_ = e_ % 2
            for kc in range(8):
                pi = kc % 2
                for a in range(3):
                    tr(PS[pi][:, :].bitcast(BF16)[:, a * 128:(a + 1) * 128], xb2[s_][:, a, kc * 128:(kc + 1) * 128], identb2, [bxb2[s_], bidb], bPS[pi])
                if kc % 2 == 0:
                    act(lambda e, pi=pi, kc=kc: e.copy(out=xbT[:, kc, :], in_=PS[pi][:, :].bitcast(BF16)[:, 0:CAP]), [bPS[pi]], [bxbT])
                else:
                    dve(lambda e, pi=pi, kc=kc: e.tensor_copy(out=xbT[:, kc, :], in_=PS[pi][:, :].bitcast(BF16)[:, 0:CAP]), [bPS[pi]], [bxbT])
            if e_ + 2 < NE:
                load_x(e_ + 2)
            for pb in range(2):
                for cc in range(4):
                    fc = pb * 4 + cc
                    r_ = fc % 2
                    pa = 2 + r_; pl = 4 + r_
                    for kc in range(8):
                        mm(PS[pa][:, 0:CAP], w1e[s_][:, kc, fc * 128:(fc + 1) * 128], xbT[:, kc, :], kc == 0, kc == 7, [bw1e[s_], bxbT], bPS[pa])
                    for kc in range(8):
                        mm(PS[pl][:, 0:CAP], w1e[s_][:, kc, D + fc * 128:D + (fc + 1) * 128], xbT[:, kc, :], kc == 0, kc == 7, [bw1e[s_], bxbT], bPS[pl])
                    dve(lambda e, r_=r_, pa=pa, fc=fc, e_=e_: e.tensor_scalar(out=g1[r_], in0=PS[pa][:, 0:CAP], scalar1=b1c[:, fc, e_:e_ + 1], scalar2=7.0, op0=ALU.add, op1=ALU.min),
                        [bPS[pa], bb1c], [bg1[r_]])
                    act(lambda e, r_=r_, pl=pl, fc=fc, e_=e_: e.activation(out=l0[r_], in_=PS[pl][:, 0:CAP], func=AF.Identity, bias=b1c[:, 8 + fc, e_:e_ + 1], scale=1.0),
                        [bPS[pl], bb1c], [bl0[r_]])
                    act(lambda e, r_=r_: e.activation(out=sg[r_], in_=g1[r_], func=AF.Sigmoid, scale=1.702), [bg1[r_]], [bsg[r_]])
                    dve(lambda e, r_=r_: e.tensor_scalar(out=l0[r_], in0=l0[r_], scalar1=7.0, scalar2=-7.0, op0=ALU.min, op1=ALU.max), [bl0[r_]], [bl0[r_]])
                    dve(lambda e, r_=r_: e.tensor_mul(out=g1[r_], in0=g1[r_], in1=sg[r_]), [bg1[r_], bsg[r_]], [bg1[r_]])
                    dve(lambda e, r_=r_, fc=fc: e.scalar_tensor_tensor(out=actT[:, fc, :], in0=l0[r_], scalar=1.0, in1=g1[r_], op0=ALU.add, op1=ALU.mult), [bl0[r_], bg1[r_]], [bact])
            if e_ + 2 < NE:
                load_w1(e_ + 2)
            for half in range(2):
                for a in range(3):
                    pi = 6 + (half * 3 + a) % 2
                    for fc in range(8):
                        mm(PS[pi][:, :], actT[:, fc, a * 128:(a + 1) * 128], w2e[s_][:, fc, half * 512:(half + 1) * 512], fc == 0, fc == 7, [bact, bw2e[s_]], bPS[pi])
                    dve(lambda e, pi=pi, a=a, half=half, s_=s_: e.tensor_add(out=y_sb[0][:, a, half * 512:(half + 1) * 512], in0=PS[pi][:, :], in1=b2row[s_][:, half * 512:(half + 1) * 512]),
                        [bPS[pi], bb2[s_]], [by[0]])
            if e_ + 2 < NE:
                load_w2(e_ + 2)
            ld(ybuf_d[e_ * CAP:(e_ + 1) * CAP, :].rearrange("(a p) d -> p a d", p=128), y_sb[0], byb, reads=[by[0]])
        P.release()
        P.barrier()

        P.mark()
        gath = [P.sb([128, D]) for _ in range(12)]; bga = [P.buf(f"ga{i}") for i in range(12)]
        xin3 = [P.sb([128, D]) for _ in range(3)]; bxin3 = [P.buf(f"xin3{i}") for i in range(3)]
        ln3 = load_ln(2)
        rt = [P.sb([128, D]) for _ in range(2)]; brt = [P.buf(f"rtc{i}") for i in range(2)]
        xo = [P.sb([128, D]) for _ in range(2)]; bxo = [P.buf(f"xoc{i}") for i in range(2)]
        bout = P.buf("out")
        def comb_load(ti):
            g3 = ti % 3
            ld(xin3[g3], x2_d[ti * 128:(ti + 1) * 128, :], bxin3[g3], reads=[bx2ds[ti]])
            for k in range(4):
                P.dma("pool", lambda e, ti=ti, k=k, g3=g3: e.indirect_dma_start(out=gath[g3 * 4 + k], out_offset=None, in_=ybuf_d,
                                                                                 in_offset=bass.IndirectOffsetOnAxis(ap=slots_all[:, ti * 4 + k:ti * 4 + k + 1], axis=0)),
                      [byb, bslot[ti]], [bga[g3 * 4 + k]])

        comb_load(0)
        comb_load(1)
        for ti in range(16):
            s_ = ti % 2
            g3 = ti % 3
            tsl = slice(ti * 128, (ti + 1) * 128)
            if ti + 2 < 16:
                comb_load(ti + 2)
            act(lambda e, s_=s_, g3=g3: e.activation(out=rt[s_], in_=xin3[g3], func=AF.Copy, scale=ALPHA), [bxin3[g3]], [brt[s_]])
            for k in range(4):
                dve(lambda e, s_=s_, k=k, ti=ti, g3=g3: e.scalar_tensor_tensor(out=rt[s_], in0=gath[g3 * 4 + k], scalar=gates_all[:, ti, k:k + 1], in1=rt[s_], op0=ALU.mult, op1=ALU.add),
                    [bga[g3 * 4 + k], bgate[ti], brt[s_]], [brt[s_]])
            layernorm(rt[s_], brt[s_], ln3[0], ln3[1], ln3[2], xo[s_], bxo[s_], par=s_, norm_on_act=True)
            ld(out_d[tsl, :], xo[s_], bout, reads=[bxo[s_]])
        P.release()
        P.barrier()
        P.emit()
    return nc


_NC_CACHE = {}


def _consts(core):
    s_ = np.arange(64)[:, None]; t_ = np.arange(64)[None, :]
    m2 = (s_ <= t_).astype(np.float32)
    m1 = m2 - (s_ <= 31).astype(np.float32)
    m3 = (s_ > t_).astype(np.float32)
    a = np.arange(128)
    inv16 = np.zeros((4, 16), np.float32)
    for g, w in enumerate((2, 4, 8, 16)):
        for j in range(16):
            inv16[g, j] = 1.0 / (min(j + 1, w) if core == 0 else w)
    return {
        "c_ident": np.eye(128, dtype=np.float32),
        "c_m12": np.ascontiguousarray(np.concatenate([m1, m2], axis=1)),
        "c_m3": m3,
        "c_ustr": (a[:, None] < a[None, :]).astype(np.float32),
        "c_ones": np.ones((128, 128), np.float32),
        "c_iota": np.ascontiguousarray(np.broadcast_to(np.arange(NE, dtype=np.float32), (128, NE))),
        "c_base": np.ascontiguousarray(np.broadcast_to(np.arange(NE, dtype=np.float32) * CAP, (128, NE))),
        "c_inv16": np.ascontiguousarray(np.broadcast_to(inv16.reshape(1, 64), (128, 64))),
    }


def kernel(**inputs):
    debug = bool(os.environ.get("MK_DEBUG"))
    f = lambda k: np.ascontiguousarray(np.asarray(inputs[k], dtype=np.float32))
    x = f("x")[0]
    shared = {
        "mem": f("mem")[0], "w_in": f("w_in")[0], "lb_logits": f("lb_logits"), "hgrn_norm_w": f("hgrn_norm_w"),
        "w_pool": f("w_pool")[0], "pool_scale": f("pool_scale").reshape(4, 128), "w_out": f("w_out")[0],
        "ln1_w": f("ln1_w"), "ln1_b": f("ln1_b"), "ln2_w": f("ln2_w"), "ln2_b": f("ln2_b"), "ln3_w": f("ln3_w"), "ln3_b": f("ln3_b"),
        "w_xq": f("w_xq")[0], "w_xk": f("w_xk")[0], "w_xv": f("w_xv")[0], "w_xo": f("w_xo")[0],
        "w_router": f("w_router")[0], "b_router": f("b_router"),
        "w1": f("w1")[0], "b1": f("b1")[0], "w2": f("w2")[0], "b2": f("b2")[0],
    }
    if debug not in _NC_CACHE:
        _NC_CACHE[debug] = build_program(debug)
    nc = _NC_CACHE[debug]
    in_maps = []
    for c in range(NCORES):
        m = dict(shared)
        m["x"] = np.ascontiguousarray(x[c * NT:(c + 1) * NT])
        m["xw"] = np.ascontiguousarray(x[c * NT - W:c * NT]) if c > 0 else np.zeros((W, D), np.float32)
        m.update(_consts(c))
        in_maps.append(m)
    res = run_bass_kernel_spmd(nc, in_maps, core_ids=list(range(NCORES)))
    out = np.concatenate([r["out"] for r in res.results], axis=0)[None]
    if debug:
        kernel.dbg = {k: np.concatenate([r[k] for r in res.results], axis=0) for k in ("x1s", "x2s")}
    return out.astype(np.float32)
```

```python
import os
from contextlib import ExitStack
import numpy as np
import concourse.bass as bass
import concourse.mybir as mybir
from concourse.bass_utils import run_bass_kernel_spmd

F32 = mybir.dt.float32
F32R = mybir.dt.float32r
I32 = mybir.dt.int32
U32 = mybir.dt.uint32
ALU = mybir.AluOpType
AF = mybir.ActivationFunctionType
AX = mybir.AxisListType


import types


def _snap(fn):
    if fn.__closure__ is None:
        return fn
    cells = []
    for c in fn.__closure__:
        try:
            cells.append(types.CellType(c.cell_contents))
        except ValueError:
            cells.append(c)
    return types.FunctionType(fn.__code__, fn.__globals__, fn.__name__, fn.__defaults__, tuple(cells))


class Buf:
    __slots__ = ("name", "w", "r", "dsem", "dcnt", "excl")

    def __init__(self, name):
        self.name = name
        self.excl = False
        self.w = None
        self.r = []
        self.dsem = None
        self.dcnt = 0


class Eng:
    def __init__(self, name, sem):
        self.name = name
        self.sem = sem
        self.cnt = 0
        self.ops = []
        self.known = {}


class Prog:
    def __init__(self, nc, stack):
        self.nc = nc
        self.stack = stack
        self.engs = {}
        self.bufs = []
        self.nsem = 0
        for n in ("pe", "act", "dve", "pool", "sp"):
            self.engs[n] = Eng(n, self.sem("e_" + n))
        self.sb_off = 0
        self.arena = None
        self.sb_marks = []
        self.uid = 0

    def sem(self, name):
        self.nsem += 1
        return self.stack.enter_context(self.nc.semaphore(name))

    def buf(self, name="b"):
        b = Buf(name)
        self.bufs.append(b)
        return b

    def sb(self, shape, dt=F32, name=None):
        if self.arena is None:
            nbytes = self.nc.sbuf_bytes_remaining
            self.arena_cols = (nbytes // 4) - 64
            self.arena = self.stack.enter_context(self.nc.sbuf_tensor("arena", [128, self.arena_cols], F32))
        n = 1
        for s_ in shape[1:]:
            n *= s_
        if dt not in (F32, F32R, I32, U32):
            n = (n + 1) // 2
        n = (n + 15) // 16 * 16
        off = self.sb_off
        self.sb_off += n
        assert self.sb_off <= self.arena_cols, ("SBUF overflow", self.sb_off, self.arena_cols)
        ap = self.arena[0:shape[0], off:off + n]
        if dt != F32:
            ap = ap.bitcast(dt)
        if len(shape) == 2:
            ap = ap[:, 0:shape[1]]
        elif len(shape) == 3:
            ap = ap[:, 0:shape[1] * shape[2]].rearrange("p (a b) -> p a b", a=shape[1], b=shape[2])
        else:
            ap = ap[:, 0:shape[1] * shape[2] * shape[3]].rearrange("p (a b c) -> p a b c", a=shape[1], b=shape[2], c=shape[3])
        return ap

    def mark(self):
        self.sb_marks.append(self.sb_off)

    def release(self):
        self.sb_off = self.sb_marks.pop()

    def _deps(self, E, reads, writes):
        deps = {}

        def add(tok):
            if tok is None:
                return
            s, v = tok
            k = id(s)
            if k not in deps or deps[k][1] < v:
                deps[k] = (s, v)

        for b in reads:
            add(b.w)
            if b.excl:
                for r in b.r:
                    if r[0] is not E.sem:
                        add(r)
        for b in writes:
            add(b.w)
            for r in b.r:
                add(r)
        for k, (s, v) in deps.items():
            if E.name == "pe" and s is E.sem:
                continue
            if E.known.get(k, 0) < v:
                E.ops.append(("w", s, v))
                E.known[k] = v

    def op(self, en, fn, reads=(), writes=()):
        E = self.engs[en]
        self._deps(E, reads, writes)
        E.cnt += 1
        E.ops.append(("o", _snap(fn), E.sem, 1))
        tok = (E.sem, E.cnt)
        for b in writes:
            b.w = tok
            b.r = []
        for b in reads:
            if b.w is not tok:
                b.r.append(tok)
        return tok

    def dma(self, en, fn, reads=(), writes=(), chain=True):
        E = self.engs[en]
        D = writes[0]
        if not chain:
            saved = D.w
            D.w = None
            self._deps(E, reads, writes)
            D.w = saved
        else:
            self._deps(E, reads, writes)
        if D.dsem is None:
            D.dsem = self.sem("d_" + D.name)
        D.dcnt += 16
        E.ops.append(("o", _snap(fn), D.dsem, 16))
        tok = (D.dsem, D.dcnt)
        for b in writes:
            b.w = tok
            b.r = []
        for b in reads:
            b.r.append(tok)
        return tok

    def barrier(self):
        toks = [(E.sem, E.cnt) for E in self.engs.values() if E.cnt > 0]
        toks += [(b.dsem, b.dcnt) for b in self.bufs if b.dsem is not None]
        for E in self.engs.values():
            for s, v in toks:
                if E.name == "pe" and s is E.sem:
                    continue
                if s is E.sem and E.name == "sp":
                    continue
                k = id(s)
                if E.known.get(k, 0) < v:
                    E.ops.append(("w", s, v))
                    E.known[k] = v
        for b in self.bufs:
            b.r = []

    def emit(self):
        with self.nc.Block() as block:
            def mk(E):
                def body(eng):
                    for o in E.ops:
                        if o[0] == "w":
                            eng.wait_ge(o[1], o[2])
                        else:
                            o[1](eng).then_inc(o[2], o[3])
                return body

            block.tensor(mk(self.engs["pe"]))
            block.scalar(mk(self.engs["act"]))
            block.vector(mk(self.engs["dve"]))
            block.gpsimd(mk(self.engs["pool"]))
            block.sync(mk(self.engs["sp"]))

NCORES = 8
S_ALL = 16384
NT = S_ALL // NCORES
D = 1024
W = 1024
NTX = NT + W
NCH = NT // 64
NWCH = W // 64
NE = 32
CAP = 384
ALPHA = 2.0 ** 0.25
BF16 = mybir.dt.bfloat16


def build_program(debug=False, stop=None, nhead=4, nseq=None):
    nc = bass.Bass("TRN2", target_bir_lowering=False)

    def din(name, shape, dt=F32):
        return nc.dram_tensor(name, list(shape), dt, kind="ExternalInput").ap()

    x_d = din("x", [NT, D]); xw_d = din("xw", [W, D]); mem_d = din("mem", [256, D])
    w_in_d = din("w_in", [D, 2560]); lbl_d = din("lb_logits", [2, 512]); hnw_d = din("hgrn_norm_w", [1, 512])
    wpool_d = din("w_pool", [4, 128, 128]); pscale_d = din("pool_scale", [4, 128]); w_out_d = din("w_out", [D, D])
    lnw_d = [din(f"ln{i}_w", [1, D]) for i in (1, 2, 3)]
    lnb_d = [din(f"ln{i}_b", [1, D]) for i in (1, 2, 3)]
    wq_d = din("w_xq", [D, D]); wk_d = din("w_xk", [D, D]); wv_d = din("w_xv", [D, D]); wo_d = din("w_xo", [D, D])
    wr_d = din("w_router", [D, NE]); br_d = din("b_router", [1, NE])
    NEW = NE if stop is None else 1
    w1_d = din("w1", [NEW, D, 2 * D]); b1_d = din("b1", [NE, 2 * D]); w2_d = din("w2", [NEW, D, D]); b2_d = din("b2", [NE, D])
    ident_d = din("c_ident", [128, 128]); m12_d = din("c_m12", [64, 128]); m3_d = din("c_m3", [64, 64])
    ustr_d = din("c_ustr", [128, 128]); ones_d = din("c_ones", [128, 128])
    iota_d = din("c_iota", [128, NE]); base_d = din("c_base", [128, NE]); inv16_d = din("c_inv16", [128, 64])
    out_d = nc.dram_tensor("out", [NT, D], F32, kind="ExternalOutput").ap()
    kind_dbg = "ExternalOutput" if debug else "Internal"
    x1_d = nc.dram_tensor("x1s", [NT, D], F32, kind=kind_dbg).ap()
    x2_d = nc.dram_tensor("x2s", [NT, D], F32, kind=kind_dbg).ap()
    mix_d = nc.dram_tensor("mixs", [8, 128, NT], BF16).ap()
    NCA = (W + NT) // 64
    hs_v = nc.dram_tensor("hs_v", [4, 64, NCA * 128], BF16).ap()
    hs_k = nc.dram_tensor("hs_k", [4, 64, NCA * 128], BF16).ap()
    hs_q = nc.dram_tensor("hs_q", [4, 128, NT], BF16).ap()
    hs_s = nc.dram_tensor("hs_s", [4, 64, (NT // 64) * 64], BF16).ap()
    hs_g = nc.dram_tensor("hs_g", [4, 64, (NT // 64) * 128], BF16).ap()
    hs_e = nc.dram_tensor("hs_e", [4, 128, NCA], F32).ap()
    xbuf_d = nc.dram_tensor("xbuf", [NE * CAP + 128, D], BF16).ap()
    ybuf_d = nc.dram_tensor("ybuf", [NE * CAP + 128, D], F32).ap()

    with ExitStack() as st:
        P = Prog(nc, st)
        PS = [st.enter_context(nc.psum_tensor(f"psb{i}", [128, 512], F32)) for i in range(8)]
        bPS = [P.buf(f"ps{i}") for i in range(8)]
        for b_ in bPS:
            b_.excl = True

        def mm(out, lhsT, rhs, start, stop, reads, wbuf):
            P.op("pe", lambda e: e.matmul(out, lhsT=lhsT, rhs=rhs, start=start, stop=stop), reads, [wbuf])

        def tr(out, in_, ident, reads, wbuf):
            P.op("pe", lambda e: e.transpose(out, in_, ident), reads, [wbuf])

        def ld(out, in_, wbuf, reads=(), q="sp", chain=True):
            P.dma(q, lambda e: e.dma_start(out=out, in_=in_), list(reads), [wbuf], chain=chain)

        def dve(fn, reads, writes):
            P.op("dve", fn, reads, writes)

        def act(fn, reads, writes):
            P.op("act", fn, reads, writes)

        ident = P.sb([128, 128]); bconst = P.buf("const")
        m12 = P.sb([64, 128]); m3 = P.sb([64, 64]); ustr = P.sb([128, 128]); ones = P.sb([128, 128])
        iota_e = P.sb([128, NE]); base_e = P.sb([128, NE]); inv16 = P.sb([128, 64])
        for t_, d_ in ((ident, ident_d), (m12, m12_d), (m3, m3_d), (ustr, ustr_d), (ones, ones_d),
                       (iota_e, iota_d), (base_e, base_d), (inv16, inv16_d)):
            ld(t_, d_, bconst, chain=False)
        brow = P.sb([128, NE]); ld(brow, br_d.broadcast_to([128, NE]), bconst, chain=False)
        lbrow = P.sb([64, 512]); omlrow = P.sb([64, 512]); hnwrow = P.sb([64, 512]); tmprow = P.sb([64, 512])
        blb = P.buf("lb")
        ld(lbrow, lbl_d[0:1, :].broadcast_to([64, 512]), blb, chain=False)
        ld(tmprow, lbl_d[1:2, :].broadcast_to([64, 512]), blb, chain=False)
        ld(hnwrow, hnw_d.broadcast_to([64, 512]), blb, chain=False)
        dve(lambda e: e.tensor_sub(out=lbrow, in0=lbrow, in1=tmprow), [blb], [blb])
        act(lambda e: e.activation(out=lbrow, in_=lbrow, func=AF.Sigmoid), [blb], [blb])
        dve(lambda e: e.tensor_scalar(out=omlrow, in0=lbrow, scalar1=-1.0, scalar2=1.0, op0=ALU.mult, op1=ALU.add), [blb], [blb])
        lb8 = P.sb([8, 128]); lbc = P.sb([128, 8]); omlc = P.sb([128, 4]); pscr = P.sb([4, 128]); pscc = P.sb([128, 4])
        ld(lb8, lbl_d.rearrange("a (h k) -> (a h) k", k=128), blb)
        ld(pscr, pscale_d, blb)
        tr(PS[0][:, 0:8], lb8, ident[0:8, 0:8], [blb, bconst], bPS[0])
        tr(PS[0][:, 8:12], pscr, ident[0:4, 0:4], [blb, bconst], bPS[0])
        dve(lambda e: e.tensor_copy(out=lbc, in_=PS[0][:, 0:8]), [bPS[0]], [blb])
        dve(lambda e: e.tensor_sub(out=lbc[:, 0:4], in0=lbc[:, 0:4], in1=lbc[:, 4:8]), [blb], [blb])
        dve(lambda e: e.tensor_copy(out=pscc, in_=PS[0][:, 8:12]), [bPS[0]], [blb])
        act(lambda e: e.activation(out=lbc[:, 0:4], in_=lbc[:, 0:4], func=AF.Sigmoid), [blb], [blb])
        dve(lambda e: e.tensor_scalar(out=omlc, in0=lbc[:, 0:4], scalar1=-1.0, scalar2=1.0, op0=ALU.mult, op1=ALU.add), [blb], [blb])
        slots_all = P.sb([128, 64], I32); gates_all = P.sb([128, 16, 4]); mask_all = P.sb([128, 16, NE])
        broute = P.buf("route")
        xin = [P.sb([128, D]) for _ in range(2)]; bxin = [P.buf(f"xin{i}") for i in range(2)]
        lnst = [(P.sb([128, 2, 6]), P.sb([128, 2]), P.sb([128, 1]), P.buf(f"lnst{i}")) for i in range(2)]
        P.mark()
        xT = P.sb([128, 8, NTX], BF16); bxT = P.buf("xT")

        def run_lockstep(gens, width=2):
            it = iter(gens)
            active = []
            for _ in range(width):
                g = next(it, None)
                if g is not None:
                    active.append(g)
            while active:
                for g in list(active):
                    try:
                        next(g)
                    except StopIteration:
                        active.remove(g)
                        n = next(it, None)
                        if n is not None:
                            active.append(n)

        for ti in range(NTX // 128):
            s_ = ti % 2
            src = xw_d[ti * 128:(ti + 1) * 128, :] if ti < W // 128 else x_d[(ti - W // 128) * 128:(ti - W // 128 + 1) * 128, :]
            ld(xin[s_], src, bxin[s_])
            for half in range(2):
                pi = (ti * 2 + half) % 4
                for j in range(4):
                    kc = half * 4 + j
                    tr(PS[pi][:, j * 128:(j + 1) * 128], xin[s_][:, kc * 128:(kc + 1) * 128], ident, [bxin[s_], bconst], bPS[pi])
                o = xT[:, half * 4:half * 4 + 4, ti * 128:(ti + 1) * 128]
                if half == 0:
                    dve(lambda e, o=o, pi=pi: e.tensor_copy(out=o, in_=PS[pi][:, :].rearrange("p (a b) -> p a b", a=4)), [bPS[pi]], [bxT])
                else:
                    act(lambda e, o=o, pi=pi: e.copy(out=o, in_=PS[pi][:, :].rearrange("p (a b) -> p a b", a=4)), [bPS[pi]], [bxT])

        w_in_v = w_in_d.rearrange("(c p) n -> p c n", p=128)

        bxb = P.buf("xbuf")
        ztile = P.sb([128, D], BF16); bzt = P.buf("ztile")
        P.op("pool", lambda e: e.memset(ztile, 0.0), [], [bzt])

        byb = P.buf("ybuf")
        bybz = P.buf("ybufz")
        ld(ybuf_d[NE * CAP:NE * CAP + 128, :], ztile, bybz, reads=[bzt], q="pool")
        ld(xbuf_d[NE * CAP:NE * CAP + 128, :], ztile, bxb, reads=[bzt], q="sp")

        def zero_fill(e0, e1):
            for e_ in range(e0, e1):
                for a in range(3):
                    ld(xbuf_d[e_ * CAP + a * 128:e_ * CAP + (a + 1) * 128, :], ztile, bxb, reads=[bzt], q="sp")

        P.mark()
        NC_ALL = NWCH + NCH
        rmask = P.sb([128, NTX], BF16); brm = P.buf("rmask")
        P.op("pool", lambda e: e.memset(rmask, 1.0), [], [brm])
        dve(lambda e: e.memset(rmask.rearrange("p (c t) -> p c t", t=64)[:, :, 0:1], 0.0), [brm], [brm])
        identb = P.sb([128, 128], BF16)
        act(lambda e: e.copy(out=identb, in_=ident), [bconst], [brm])
        nomlc = P.sb([128, 4])
        dve(lambda e: e.tensor_scalar(out=nomlc, in0=omlc, scalar1=-1.0, scalar2=None, op0=ALU.mult), [blb], [brm])
        wfms = [P.sb([128, 8, 256], BF16) for _ in range(2)]; bwfms = [P.buf(f"wfm{i}") for i in range(2)]
        wtks = [P.sb([128, 8, 256], BF16) for _ in range(2)]; bwtks = [P.buf(f"wtk{i}") for i in range(2)]
        q_fm = P.sb([128, NT]); bqf = P.buf("qfm")
        zb = P.sb([128, NTX]); bzb = P.buf("zb")
        keyf = P.sb([128, NTX]); bkf = P.buf("keyf")
        bcum = P.sb([128, NTX]); bbc = P.buf("bcum")
        scr2 = P.sb([128, NT]); bscr2 = P.buf("scr2")
        q_inter = P.sb([128, NT], BF16); bqn = P.buf("qinter")
        qki = P.sb([128, 2 * NT], BF16); bqki = P.buf("qki")
        q_intra = qki[:, 0:NT]; k_intra = qki[:, NT:2 * NT]; kst_fm = qki[:, 0:NTX]
        v_tok = P.sb([64, NC_ALL, 128], BF16); k_state = P.sb([64, NC_ALL, 128], BF16); bvt = P.buf("vt"); bks = P.buf("ks")
        g_tok = P.sb([64, NCH, 128], BF16); bg = P.buf("g")
        eb_last = P.sb([128, NC_ALL]); beb = P.buf("eb")
        scT = P.sb([64, NCH, 64], BF16); bsc = P.buf("scT")
        S32 = [P.sb([128, 128]) for _ in range(2)]; Sbf = [P.sb([128, 128], BF16) for _ in range(2)]
        bS = [P.buf(f"S{i}") for i in range(2)]; bSb = [P.buf(f"Sbf{i}") for i in range(2)]
        bds = [P.buf(f"ds{i}") for i in range(8)]
        for b_ in bds:
            b_.excl = True
        o_sbs = [scr2[0:64, 0:1024].rearrange("p (a b) -> p a b", a=8), scr2[0:64, 1024:2048].rearrange("p (a b) -> p a b", a=8)]; bos = [P.buf(f"o{i}") for i in range(2)]; tsq = P.sb([64, 8, 128]); btsq = P.buf("tsq")
        ssq = P.sb([64, 8]); rstd = P.sb([64, 8]); bss = P.buf("ss")
        stg = [P.sb([128, 512], BF16) for _ in range(2)]; bstg = [P.buf(f"stg{i}") for i in range(2)]
        bmixd = P.buf("mixd")
        mask64 = m12[:, 64:128]
        zb3 = zb.rearrange("p (c t) -> p c t", t=64); bc3 = bcum.rearrange("p (c t) -> p c t", t=64)
        nstg = 0

        def load_head_w(hh):
            wf = wfms[hh % 2]; bwf = bwfms[hh % 2]; wt = wtks[hh % 2]; bwt = bwtks[hh % 2]
            ld(wf[:, :, 0:128], w_in_v[:, :, hh * 128:(hh + 1) * 128], bwf, q="pool")
            ld(wf[:, :, 128:256], w_in_v[:, :, 512 + hh * 128:512 + (hh + 1) * 128], bwf, q="pool")
            ld(wt[:, :, 0:128], w_in_v[:, :, 1024 + hh * 128:1024 + (hh + 1) * 128], bwt, q="pool")
            ld(wt[:, :, 128:256], w_in_v[:, :, 1536 + hh * 128:1536 + (hh + 1) * 128], bwt, q="pool")

        bhs = [P.buf(f"hs{i}") for i in range(4)]
        bzbh = [P.buf(f"zbh{i}") for i in range(2)]; bbch = [P.buf(f"bch{i}") for i in range(2)]; bkfh = [P.buf(f"kfh{i}") for i in range(2)]
        bscr2h = [P.buf(f"s2h{i}") for i in range(2)]; bqkih = [P.buf(f"qkh{i}") for i in range(2)]; bkst = [P.buf(f"kst{i}") for i in range(2)]
        bebh = [P.buf(f"ebh{i}") for i in range(2)]; bqnh = [P.buf(f"qnh{i}") for i in range(2)]; bsch = [P.buf(f"sch{i}") for i in range(2)]
        for h in range(nhead):
            wfm = wfms[h % 2]; bwfm = bwfms[h % 2]; wtk = wtks[h % 2]; bwtk = bwtks[h % 2]
            if h == 0:
                load_head_w(0)
            zero_fill(h * 8, h * 8 + 8)
            for tb in range(4):
                pi = tb % 2
                for kc in range(8):
                    mm(PS[pi][:, :], wfm[:, kc, 0:128], xT[:, kc, W + tb * 512:W + (tb + 1) * 512], kc == 0, kc == 7, [bwfm, bxT], bPS[pi])
                dve(lambda e, pi=pi, tb=tb: e.tensor_copy(out=q_fm[:, tb * 512:(tb + 1) * 512], in_=PS[pi][:, :]), [bPS[pi]], [bqf])
            for tb in range(NTX // 512):
                pi = 2 + tb % 2
                for kc in range(8):
                    mm(PS[pi][:, :], wfm[:, kc, 128:256], xT[:, kc, tb * 512:(tb + 1) * 512], kc == 0, kc == 7, [bwfm, bxT], bPS[pi])
                act(lambda e, pi=pi, tb=tb: e.activation(out=zb[:, tb * 512:(tb + 1) * 512], in_=PS[pi][:, :], func=AF.Exp, scale=-1.0), [bPS[pi]], [bzb, bzbh[0], bzbh[1]])
            if stop == "C1":
                P.barrier(); P.emit(); return nc
            for c2 in range(NC_ALL // 2):
                pi = 4 + c2 % 4
                own2 = c2 * 2 - NWCH
                ncol = 256 if own2 >= 0 else 128
                for j in range(2):
                    c = c2 * 2 + j
                    for kc in range(8):
                        mm(PS[pi][0:64, j * 256:j * 256 + ncol], xT[:, kc, c * 64:(c + 1) * 64], wtk[:, kc, 0:ncol], kc == 0, kc == 7, [bwtk, bxT], bPS[pi])
                pv = PS[pi][0:64, :].rearrange("p (a b) -> p a b", a=2)
                dve(lambda e, pv=pv, c2=c2: e.tensor_copy(out=v_tok[:, c2 * 2:c2 * 2 + 2, :], in_=pv[:, :, 0:128]), [bPS[pi]], [bvt])
                if own2 >= 0:
                    act(lambda e, pv=pv, own2=own2: e.activation(out=g_tok[:, own2:own2 + 2, :], in_=pv[:, :, 128:256], func=AF.Silu), [bPS[pi]], [bg])
            if stop == "C2":
                P.barrier(); P.emit(); return nc
            P.op("pool", lambda e, h=h: e.tensor_mul(out=g_tok, in0=g_tok, in1=hnwrow[:, h * 128:(h + 1) * 128].unsqueeze(1).broadcast_to([64, NCH, 128])), [bg, blb], [bg])
            def chain_half(hf, h=h):
                HT = NTX // 2
                t0 = hf * HT; t1 = t0 + HT
                c0 = t0 // 64; c1 = t1 // 64
                o0 = max(t0, W) - W; o1 = t1 - W
                oc0 = o0 // 64; oc1 = o1 // 64
                ts_ = slice(t0, t1); tow = slice(W + o0, W + o1); to = slice(o0, o1)
                zbh = bzbh[hf]; bch = bbch[hf]; kfh = bkfh[hf]; s2h = bscr2h[hf]; qkh = bqkih[hf]
                act(lambda e: e.activation(out=zb[:, ts_], in_=zb[:, ts_], func=AF.Ln, bias=1.0, scale=1.0), [bzb, zbh], [zbh])
                yield
                act(lambda e: e.activation(out=zb[:, ts_], in_=zb[:, ts_], func=AF.Exp, scale=-1.0), [zbh], [zbh])
                yield
                dve(lambda e: e.tensor_scalar(out=bcum[:, ts_], in0=zb[:, ts_], scalar1=omlc[:, h:h + 1], scalar2=lbc[:, h:h + 1], op0=ALU.mult, op1=ALU.add), [zbh, blb], [bch])
                dve(lambda e: e.tensor_scalar(out=keyf[:, ts_], in0=zb[:, ts_], scalar1=nomlc[:, h:h + 1], scalar2=omlc[:, h:h + 1], op0=ALU.mult, op1=ALU.add), [zbh, blb, brm], [kfh])
                yield
                act(lambda e: e.activation(out=bcum[:, ts_], in_=bcum[:, ts_], func=AF.Ln), [bch], [bch])
                yield
                dve(lambda e: e.tensor_tensor_scan(out=zb[:, ts_], data0=rmask[:, ts_], data1=bcum[:, ts_], initial=0.0, op0=ALU.mult, op1=ALU.add), [brm, bch, zbh], [zbh])
                yield
                act(lambda e: e.activation(out=bcum[:, ts_], in_=zb[:, ts_], func=AF.Exp), [zbh, bch], [bch])
                yield
                dve(lambda e: e.tensor_copy(out=eb_last[:, c0:c1], in_=bc3[:, c0:c1, 63]), [bch], [bebh[hf]])
                dve(lambda e: e.tensor_mul(out=q_inter[:, to], in0=q_fm[:, to], in1=bcum[:, tow]), [bch, bqf], [bqnh[hf]])
                dve(lambda e: e.tensor_sub(out=bc3[:, NWCH + oc0:NWCH + oc1, :], in0=zb3[:, NWCH + oc0:NWCH + oc1, :],
                                            in1=zb3[:, NWCH + oc0:NWCH + oc1, 31:32].broadcast_to([128, oc1 - oc0, 64])), [zbh, bch, bebh[hf], bqnh[hf]], [bch])
                yield
                act(lambda e: e.activation(out=scr2[:, to], in_=bcum[:, tow], func=AF.Exp), [bch], [s2h])
                yield
                dve(lambda e: e.tensor_mul(out=q_intra[:, to], in0=q_fm[:, to], in1=scr2[:, to]), [s2h, bqf], [qkh])
                act(lambda e: e.activation(out=scr2[:, to], in_=bcum[:, tow], func=AF.Exp, scale=-1.0), [bch, s2h], [s2h])
                yield
                dve(lambda e: e.tensor_mul(out=k_intra[:, to], in0=keyf[:, tow], in1=scr2[:, to]), [s2h, kfh], [qkh])
                yield
                for g8 in range(oc0 // 8, oc1 // 8):
                    pi = hf
                    for cj in range(8):
                        c = g8 * 8 + cj
                        mm(PS[pi][0:64, cj * 64:(cj + 1) * 64], k_intra[:, c * 64:(c + 1) * 64], q_intra[:, c * 64:(c + 1) * 64], True, True, [qkh], bPS[pi])
                    yield
                    dve(lambda e, pi=pi, g8=g8: e.tensor_mul(out=scT[:, g8 * 8:(g8 + 1) * 8, :], in0=PS[pi][0:64, :].rearrange("p (a b) -> p a b", a=8),
                                                          in1=mask64.unsqueeze(1).broadcast_to([64, 8, 64])), [bPS[pi], bconst], [bsch[hf]])
                dve(lambda e: e.tensor_sub(out=bc3[:, c0:c1, :], in0=zb3[:, c0:c1, 63:64].broadcast_to([128, c1 - c0, 64]), in1=zb3[:, c0:c1, :]), [zbh, bch], [bch])
                yield
                act(lambda e: e.activation(out=bcum[:, ts_], in_=bcum[:, ts_], func=AF.Exp), [bch], [bch])
                yield

            run_lockstep([chain_half(0), chain_half(1)])
            for hf in range(2):
                HT = NTX // 2
                ts_ = slice(hf * HT, (hf + 1) * HT)
                dve(lambda e, ts_=ts_: e.tensor_mul(out=kst_fm[:, ts_], in0=keyf[:, ts_], in1=bcum[:, ts_]), [bbch[hf], bkfh[hf], bqkih[0], bqkih[1]], [bkst[hf], bqkih[0], bqkih[1]])
            for g4 in range(NC_ALL // 4):
                pi = 2 + g4 % 2
                hf = 0 if g4 * 4 < NC_ALL // 2 else 1
                for cj in range(4):
                    c = g4 * 4 + cj
                    mm(PS[pi][0:64, cj * 128:(cj + 1) * 128], kst_fm[:, c * 64:(c + 1) * 64], identb, True, True, [bkst[hf], brm], bPS[pi])
                o_ = k_state[:, g4 * 4:g4 * 4 + 4, :]
                if g4 % 2 == 0:
                    act(lambda e, pi=pi, o_=o_: e.copy(out=o_, in_=PS[pi][0:64, :].rearrange("p (a b) -> p a b", a=4)), [bPS[pi]], [bks])
                else:
                    dve(lambda e, pi=pi, o_=o_: e.tensor_copy(out=o_, in_=PS[pi][0:64, :].rearrange("p (a b) -> p a b", a=4)), [bPS[pi]], [bks])
            if stop == "C6":
                P.barrier(); P.emit(); return nc
            if h + 1 < nhead:
                load_head_w(h + 1)
            ld(hs_v[h], v_tok.rearrange("p a b -> p (a b)"), bhs[h], reads=[bvt])
            ld(hs_k[h], k_state.rearrange("p a b -> p (a b)"), bhs[h], reads=[bks])
            ld(hs_q[h], q_inter, bhs[h], reads=bqnh)
            ld(hs_s[h], scT.rearrange("p a b -> p (a b)"), bhs[h], reads=bsch)
            ld(hs_g[h], g_tok.rearrange("p a b -> p (a b)"), bhs[h], reads=[bg])
            ld(hs_e[h], eb_last, bhs[h], reads=bebh)
        P.release()
        P.barrier()

        if stop == "C":
            P.emit()
            return nc
        P.mark()
        NP = NT + 16
        wp = [P.sb([128, 8, 128], BF16) for _ in range(2)]; bwp = [P.buf(f"wp{i}") for i in range(2)]
        wpl = [P.sb([128, 128], BF16) for _ in range(2)]; bwpl = [P.buf(f"wpl{i}") for i in range(2)]
        p_sbs = [P.sb([128, NP]) for _ in range(2)]; sas = [P.sb([128, NP]) for _ in range(2)]; sbbs = [P.sb([128, NP]) for _ in range(2)]
        bps_ = [P.buf(f"p{i}") for i in range(2)]; bsas = [P.buf(f"sa{i}") for i in range(2)]; bsbs = [P.buf(f"sb{i}") for i in range(2)]
        d_bfs = [P.sb([128, NT], BF16) for _ in range(2)]; bds_ = [P.buf(f"d{i}") for i in range(2)]
        t16s = [P.sb([128, 16]) for _ in range(2)]; bt16s = [P.buf(f"t16{i}") for i in range(2)]
        stg2 = [P.sb([128, 512], BF16) for _ in range(4)]; bstg2 = [P.buf(f"stgp{i}") for i in range(4)]

        def group_D(gi, w):
            s_ = gi % 2
            B0 = 4 * s_
            p_sb = p_sbs[s_]; sa = sas[s_]; sbb = sbbs[s_]; bp = bps_[s_]; bsa = bsas[s_]; bsb2 = bsbs[s_]
            d_bf = d_bfs[s_]; bd = bds_[s_]; t16 = t16s[s_]; bt16 = bt16s[s_]
            ld(wp[s_], w_in_v[:, :, 2048 + gi * 128:2048 + (gi + 1) * 128], bwp[s_], q="pool")
            ld(wpl[s_], wpool_d[gi], bwpl[s_], q="pool")
            for tb in range(5):
                pi = B0 + tb % 4
                c0 = W - 16 + tb * 512; n = 512 if tb < 4 else 16
                for kc in range(8):
                    mm(PS[pi][:, 0:n], wp[s_][:, kc, :], xT[:, kc, c0:c0 + n], kc == 0, kc == 7, [bwp[s_], bxT], bPS[pi])
                yield
                act(lambda e, pi=pi, tb=tb, n=n: e.copy(out=p_sb[:, tb * 512:tb * 512 + n], in_=PS[pi][:, 0:n]), [bPS[pi]], [bp])
            cur, bcur = p_sb, bp
            step = 1
            bufs2 = [(sa, bsa), (sbb, bsb2)]
            k = 0
            while step < w:
                nxt, bnxt = bufs2[k % 2]
                yield
                dve(lambda e, cur=cur, nxt=nxt, step=step: e.tensor_add(out=nxt[:, step:NP], in0=cur[:, step:NP], in1=cur[:, 0:NP - step]), [bcur], [bnxt])
                cur, bcur = nxt, bnxt
                step *= 2; k += 1
            yield
            dve(lambda e, cur=cur, w=w: e.scalar_tensor_tensor(out=d_bf, in0=cur[:, 16:NP], scalar=1.0 / w, in1=p_sb[:, 16:NP], op0=ALU.mult, op1=ALU.subtract), [bcur, bp], [bd])
            dve(lambda e, cur=cur, gi=gi: e.tensor_mul(out=t16, in0=cur[:, 16:32], in1=inv16[:, gi * 16:(gi + 1) * 16]), [bcur, bconst], [bt16])
            dve(lambda e: e.tensor_sub(out=d_bf[:, 0:16], in0=t16, in1=p_sb[:, 16:32]), [bt16, bp, bd], [bd])
            for tb in range(4):
                pi = B0 + tb % 4
                mm(PS[pi][:, :], wpl[s_], d_bf[:, tb * 512:(tb + 1) * 512], True, True, [bwpl[s_], bd], bPS[pi])
                sg_ = 2 * s_ + tb % 2
                yield
                dve(lambda e, pi=pi, sg_=sg_, gi=gi: e.tensor_scalar(out=stg2[sg_], in0=PS[pi][:, :], scalar1=pscc[:, gi:gi + 1], scalar2=None, op0=ALU.mult),
                    [bPS[pi], blb], [bstg2[sg_]])
                ld(mix_d[4 + gi, :, tb * 512:(tb + 1) * 512], stg2[sg_], bmixds[4 + gi], reads=[bstg2[sg_]])
            yield

        bmixds = [None] * 4 + [P.buf("mixdp")] * 4
        run_lockstep([group_D(gi, w) for gi, w in enumerate((2, 4, 8, 16))])
        P.release()
        P.barrier()

        P.release()
        P.mark()
        P.mark()
        NC_ALL = NWCH + NCH
        R_v = [P.sb([64, NC_ALL, 128], BF16) for _ in range(2)]; R_k = [P.sb([64, NC_ALL, 128], BF16) for _ in range(2)]
        R_q = [P.sb([128, NT], BF16) for _ in range(2)]; R_s = [P.sb([64, NCH, 64], BF16) for _ in range(2)]
        R_g = [P.sb([64, NCH, 128], BF16) for _ in range(2)]; R_e = [P.sb([128, NC_ALL]) for _ in range(2)]
        bR = [P.buf(f"R{i}") for i in range(2)]
        R_S32 = [[P.sb([128, 128]) for _ in range(2)] for _ in range(2)]; R_Sbf = [[P.sb([128, 128], BF16) for _ in range(2)] for _ in range(2)]
        bRS = [[P.buf(f"RS{i}{j}") for j in range(2)] for i in range(2)]; bRSb = [[P.buf(f"RSb{i}{j}") for j in range(2)] for i in range(2)]
        R_o = [[P.sb([64, 8, 128]) for _ in range(2)] for _ in range(2)]; bRo = [[P.buf(f"Ro{i}{j}") for j in range(2)] for i in range(2)]
        R_tsq = [P.sb([64, 8, 128]) for _ in range(2)]; R_ssq = [P.sb([64, 8]) for _ in range(2)]; R_rstd = [P.sb([64, 8]) for _ in range(2)]
        bRss = [P.buf(f"Rss{i}") for i in range(2)]; bRtsq = [P.buf(f"Rtsq{i}") for i in range(2)]
        R_stg = [[P.sb([128, 512], BF16) for _ in range(2)] for _ in range(2)]; bRstg = [[P.buf(f"Rstg{i}{j}") for j in range(2)] for i in range(2)]
        bmixh = [P.buf(f"mixh{i}") for i in range(4)]

        def rec_head(h, st):
            B0 = 4 * st
            v_tok = R_v[st]; k_state = R_k[st]; q_inter = R_q[st]; scT = R_s[st]; g_tok = R_g[st]; eb_last = R_e[st]
            S32 = R_S32[st]; Sbf = R_Sbf[st]; bS = bRS[st]; bSb = bRSb[st]
            tsq = R_tsq[st]; ssq = R_ssq[st]; rstd = R_rstd[st]; bss = bRss[st]; btsq = bRtsq[st]
            ld(v_tok.rearrange("p a b -> p (a b)"), hs_v[h], bR[st], reads=[bhs[h]])
            ld(k_state.rearrange("p a b -> p (a b)"), hs_k[h], bR[st], reads=[bhs[h]])
            ld(q_inter, hs_q[h], bR[st], reads=[bhs[h]])
            ld(scT.rearrange("p a b -> p (a b)"), hs_s[h], bR[st], reads=[bhs[h]])
            ld(g_tok.rearrange("p a b -> p (a b)"), hs_g[h], bR[st], reads=[bhs[h]])
            ld(eb_last, hs_e[h], bR[st], reads=[bhs[h]])
            dve(lambda e: e.memset(S32[0], 0.0), [], [bS[0]])
            dve(lambda e: e.memset(Sbf[0], 0.0), [], [bSb[0]])
            yield

            def issue_ds_group(g):
                pd = B0 + 1 + g % 2
                for c in range(g * 4, g * 4 + 4):
                    mm(PS[pd][:, (c % 4) * 128:(c % 4 + 1) * 128], k_state[:, c, :], v_tok[:, c, :], True, True, [bR[st]], bPS[pd])

            issue_ds_group(0)
            nstg = 0
            for c in range(NC_ALL):
                own = c - NWCH
                if c % 4 == 0 and c // 4 + 1 < NC_ALL // 4:
                    issue_ds_group(c // 4 + 1)
                cur = c % 2; nxt = (c + 1) % 2
                if own >= 0:
                    o_sb = R_o[st][(own // 8) % 2]; bo = bRo[st][(own // 8) % 2]
                    po = B0
                    osl = PS[po][0:64, (own % 4) * 128:(own % 4 + 1) * 128]
                    mm(osl, q_inter[:, own * 64:(own + 1) * 64], Sbf[cur], True, False, [bR[st], bSb[cur]], bPS[po])
                    mm(osl, scT[:, own, :], v_tok[:, c, :], False, True, [bR[st]], bPS[po])
                    if own % 4 == 3:
                        g4 = (own // 4) % 2
                        act(lambda e, po=po, g4=g4, o_sb=o_sb: e.copy(out=o_sb[:, g4 * 4:g4 * 4 + 4, :], in_=PS[po][0:64, :].rearrange("p (a b) -> p a b", a=4)), [bPS[po]], [bo])
                pd = B0 + 1 + (c // 4) % 2
                dve(lambda e, c=c, pd=pd, cur=cur, nxt=nxt: e.scalar_tensor_tensor(out=S32[nxt], in0=S32[cur], scalar=eb_last[:, c:c + 1], in1=PS[pd][:, (c % 4) * 128:(c % 4 + 1) * 128], op0=ALU.mult, op1=ALU.add),
                    [bS[cur], bR[st], bPS[pd]], [bS[nxt]])
                if c >= NWCH - 1 and c < NC_ALL - 1:
                    act(lambda e, nxt=nxt: e.copy(out=Sbf[nxt], in_=S32[nxt]), [bS[nxt]], [bSb[nxt]])
                yield
                if own >= 0 and own % 8 == 7:
                    g8 = own // 8
                    gsl = slice(g8 * 8, g8 * 8 + 8)
                    dve(lambda e, o_sb=o_sb: e.tensor_mul(out=tsq, in0=o_sb, in1=o_sb), [bo], [btsq])
                    dve(lambda e: e.tensor_reduce(out=ssq, in_=tsq, axis=AX.X, op=ALU.add), [btsq], [bss])
                    act(lambda e: e.activation(out=ssq, in_=ssq, func=AF.Ln, bias=1e-6, scale=1.0 / 128.0), [bss], [bss])
                    act(lambda e: e.activation(out=rstd, in_=ssq, func=AF.Exp, scale=-0.5), [bss], [bss])
                    yield
                    dve(lambda e, o_sb=o_sb: e.tensor_mul(out=o_sb, in0=o_sb, in1=rstd.unsqueeze(2).broadcast_to([64, 8, 128])), [bo, bss], [bo])
                    P.op("pool", lambda e, gsl=gsl, o_sb=o_sb: e.tensor_mul(out=o_sb, in0=o_sb, in1=g_tok[:, gsl, :]), [bo, bR[st]], [bo])
                    yield
                    pi = B0 + 3
                    for cj in range(8):
                        tr(PS[pi][:, cj * 64:(cj + 1) * 64], o_sb[:, cj, :], ident[0:64, 0:64], [bo, bconst], bPS[pi])
                    sg_ = nstg % 2; nstg += 1
                    yield
                    act(lambda e, pi=pi, sg_=sg_: e.copy(out=R_stg[st][sg_], in_=PS[pi][:, :]), [bPS[pi]], [bRstg[st][sg_]])
                    ld(mix_d[h, :, g8 * 512:(g8 + 1) * 512], R_stg[st][sg_], bmixh[h], reads=[bRstg[st][sg_]])
            yield

        if nhead == 4:
            run_lockstep([rec_head(0, 0), rec_head(1, 1)])
            run_lockstep([rec_head(2, 0), rec_head(3, 1)])
        P.release()
        P.barrier()
        if stop == "C2":
            P.emit()
            return nc

        def layernorm(r, br, lw, lb_, blw, out, bout, par=0, use_pool=False, norm_on_act=False):
            stats, mv, rs, bst = lnst[par]
            dve(lambda e: e.bn_stats(out=stats[:, 0, :], in_=r[:, 0:512]), [br], [bst])
            dve(lambda e: e.bn_stats(out=stats[:, 1, :], in_=r[:, 512:1024]), [br], [bst])
            dve(lambda e: e.bn_aggr(out=mv, in_=stats[:, :, :].rearrange("p a b -> p (a b)")), [bst], [bst])
            act(lambda e: e.activation(out=rs, in_=mv[:, 1:2], func=AF.Ln, bias=1e-5, scale=1.0), [bst], [bst])
            act(lambda e: e.activation(out=rs, in_=rs, func=AF.Exp, scale=-0.5), [bst], [bst])
            if norm_on_act:
                dve(lambda e: e.scalar_tensor_tensor(out=mv[:, 1:2], in0=mv[:, 0:1], scalar=-1.0, in1=rs, op0=ALU.mult, op1=ALU.mult), [bst], [bst])
                act(lambda e: e.activation(out=r, in_=r, func=AF.Identity, bias=mv[:, 1:2], scale=rs[:, 0:1]), [br, bst], [br])
            else:
                dve(lambda e: e.tensor_scalar(out=r, in0=r, scalar1=mv[:, 0:1], scalar2=rs[:, 0:1], op0=ALU.subtract, op1=ALU.mult), [br, bst], [br])
            eng_ = "pool" if use_pool else "dve"
            P.op(eng_, lambda e: e.tensor_mul(out=r, in0=r, in1=lw), [br, blw], [br])
            P.op(eng_, lambda e: e.tensor_add(out=out, in0=r, in1=lb_), [br, blw], [bout])

        def load_ln(i):
            lw = P.sb([128, D]); lb_ = P.sb([128, D]); blw = P.buf(f"ln{i}")
            ld(lw, lnw_d[i].broadcast_to([128, D]), blw)
            ld(lb_, lnb_d[i].broadcast_to([128, D]), blw)
            return lw, lb_, blw

        P.mark()
        ln1 = load_ln(0); ln2 = load_ln(1)
        mixT = P.sb([128, 8, NT], BF16); bmixT = P.buf("mixT")
        ld(mixT, mix_d.rearrange("c p t -> p c t"), bmixT, reads=bmixh + bmixds[4:5])
        wbig = [P.sb([128, 8, D], BF16) for _ in range(2)]; bwbig = [P.buf(f"wbig{i}") for i in range(2)]
        ld(wbig[0], w_out_d.rearrange("(c p) n -> p c n", p=128), bwbig[0], q="pool")
        x1T = P.sb([128, 8, NT], BF16)
        rt = [P.sb([128, D]) for _ in range(2)]; brt = [P.buf(f"rt{i}") for i in range(2)]
        xo = [P.sb([128, D]) for _ in range(2)]; bxo = [P.buf(f"xo{i}") for i in range(2)]
        bx1ds = [P.buf(f"x1d{i}") for i in range(16)]; bx2ds = [P.buf(f"x2d{i}") for i in range(16)]
        bx1Ts = [P.buf(f"x1T{i}") for i in range(16)]

        def tile_E(ti):
            s_ = ti % 2
            B0 = 4 * s_
            ld(xin[s_], x_d[ti * 128:(ti + 1) * 128, :], bxin[s_])
            for half in range(2):
                pi = B0 + half
                for kc in range(8):
                    mm(PS[pi][:, :], mixT[:, kc, ti * 128:(ti + 1) * 128], wbig[0][:, kc, half * 512:(half + 1) * 512], kc == 0, kc == 7, [bmixT, bwbig[0]], bPS[pi])
                yield
                dve(lambda e, s_=s_, pi=pi, half=half: e.scalar_tensor_tensor(out=rt[s_][:, half * 512:(half + 1) * 512], in0=xin[s_][:, half * 512:(half + 1) * 512], scalar=ALPHA,
                                                                         in1=PS[pi][:, :], op0=ALU.mult, op1=ALU.add), [bxin[s_], bPS[pi]], [brt[s_]])
            yield
            layernorm(rt[s_], brt[s_], ln1[0], ln1[1], ln1[2], xo[s_], bxo[s_], par=s_, use_pool=True)
            yield
            ld(x1_d[ti * 128:(ti + 1) * 128, :], xo[s_], bx1ds[ti], reads=[bxo[s_]])
            for half in range(2):
                pi = B0 + 2 + half
                for j in range(4):
                    kc = half * 4 + j
                    tr(PS[pi][:, j * 128:(j + 1) * 128], xo[s_][:, kc * 128:(kc + 1) * 128], ident, [bxo[s_], bconst], bPS[pi])
                o = x1T[:, half * 4:half * 4 + 4, ti * 128:(ti + 1) * 128]
                yield
                act(lambda e, o=o, pi=pi: e.copy(out=o, in_=PS[pi][:, :].rearrange("p (a b) -> p a b", a=4)), [bPS[pi]], [bx1Ts[ti]])
            yield

        run_lockstep([tile_E(ti) for ti in range(16)])
        P.barrier()

        qT = mixT; bqT = bmixT
        ld(wbig[1], wk_d.rearrange("(c p) n -> p c n", p=128), bwbig[1], q="pool")
        kT = P.sb([128, 8, 256], BF16); bkT = P.buf("kT"); v_m = P.sb([128, 2, D], BF16); bvm = P.buf("vm")
        P.mark()
        memT = P.sb([128, 8, 256], BF16); bmemT = P.buf("memT")
        P.release()
        for mt in range(2):
            ld(xin[mt], mem_d[mt * 128:(mt + 1) * 128, :], bxin[mt])
            for half in range(2):
                pi = 4 + half
                for j in range(4):
                    kc = half * 4 + j
                    tr(PS[pi][:, j * 128:(j + 1) * 128], xin[mt][:, kc * 128:(kc + 1) * 128], ident, [bxin[mt], bconst], bPS[pi])
                o = memT[:, half * 4:half * 4 + 4, mt * 128:(mt + 1) * 128]
                dve(lambda e, o=o, pi=pi: e.tensor_copy(out=o, in_=PS[pi][:, :].rearrange("p (a b) -> p a b", a=4)), [bPS[pi]], [bmemT])
        for fc in range(8):
            pi = fc % 2
            for kc in range(8):
                mm(PS[pi][:, 0:256], wbig[1][:, kc, fc * 128:(fc + 1) * 128], memT[:, kc, :], kc == 0, kc == 7, [bwbig[1], bmemT], bPS[pi])
            act(lambda e, pi=pi, fc=fc: e.copy(out=kT[:, fc, :], in_=PS[pi][:, 0:256]), [bPS[pi]], [bkT])
        ld(wbig[0], wv_d.rearrange("(c p) n -> p c n", p=128), bwbig[0], q="pool")
        for mt in range(2):
            for half in range(2):
                pi = 2 + half
                for kc in range(8):
                    mm(PS[pi][:, :], memT[:, kc, mt * 128:(mt + 1) * 128], wbig[0][:, kc, half * 512:(half + 1) * 512], kc == 0, kc == 7, [bwbig[0], bmemT], bPS[pi])
                act(lambda e, pi=pi, mt=mt, half=half: e.copy(out=v_m[:, mt, half * 512:(half + 1) * 512], in_=PS[pi][:, :]), [bPS[pi]], [bvm])
        ld(wbig[1], wq_d.rearrange("(c p) n -> p c n", p=128), bwbig[1], q="pool")
        for fc in range(8):
            for tb in range(4):
                pi = (fc * 4 + tb) % 4
                for kc in range(8):
                    mm(PS[pi][:, :], wbig[1][:, kc, fc * 128:(fc + 1) * 128], x1T[:, kc, tb * 512:(tb + 1) * 512], kc == 0, kc == 7, [bwbig[1]] + bx1Ts[tb * 4:tb * 4 + 4], bPS[pi])
                if tb % 2 == 0:
                    act(lambda e, pi=pi, fc=fc, tb=tb: e.activation(out=qT[:, fc, tb * 512:(tb + 1) * 512], in_=PS[pi][:, :], func=AF.Copy, scale=1.0 / 16.0), [bPS[pi]], [bqT])
                else:
                    dve(lambda e, pi=pi, fc=fc, tb=tb: e.tensor_scalar(out=qT[:, fc, tb * 512:(tb + 1) * 512], in0=PS[pi][:, :], scalar1=1.0 / 16.0, scalar2=None, op0=ALU.mult), [bPS[pi]], [bqT])
        ld(wbig[0], wo_d.rearrange("(c p) n -> p c n", p=128), bwbig[0], q="pool")
        wr32 = P.sb([128, 8, NE]); bwr = P.buf("wr")
        P.dma("sp", lambda e: e.dma_start(out=wr32, in_=wr_d.rearrange("(c p) n -> p c n", p=128)), [], [bwr, bmemT])
        pexp = [P.sb([128, 4, 256]) for _ in range(2)]; bpe = [P.buf(f"pexp{i}") for i in range(2)]
        nmx = [P.sb([128, 12]) for _ in range(2)]; bnm = [P.buf(f"nmx{i}") for i in range(2)]
        pT = [P.sb([128, 8, 128], BF16) for _ in range(2)]; bpT = [P.buf(f"pT{i}") for i in range(2)]
        oT = [P.sb([128, 8, 128], BF16) for _ in range(2)]; boT = [P.buf(f"oT{i}") for i in range(2)]
        x2T = [P.sb([128, 8, 128]) for _ in range(2)]; bx2T = [P.buf(f"x2T{i}") for i in range(2)]
        lgs = [P.sb([128, NE]) for _ in range(2)]; top8s = [P.sb([128, 8]) for _ in range(2)]; idx8s = [P.sb([128, 8], U32) for _ in range(2)]
        idxfs = [P.sb([128, 4]) for _ in range(2)]; blgs = [P.buf(f"lg{i}") for i in range(2)]
        gexs = [P.sb([128, 6]) for _ in range(2)]; rksls = [P.sb([128, NE]) for _ in range(2)]; ohss = [P.sb([128, NE]) for _ in range(2)]
        slotfs = [P.sb([128, 4]) for _ in range(2)]; brks = [P.buf(f"rk{i}") for i in range(2)]
        bmask = [P.buf(f"mask{i}") for i in range(16)]; bslot = [P.buf(f"slot{i}") for i in range(16)]; bgate = [P.buf(f"gate{i}") for i in range(16)]

        bxbk = [P.buf(f"xbk{i}") for i in range(4)]
        xob = [P.sb([128, D], BF16) for _ in range(2)]; bxob = [P.buf(f"xob{i}") for i in range(2)]
        cum_all = P.sb([128, 16, NE]); bcumm = [P.buf(f"cum{i}") for i in range(16)]
        dve(lambda e: e.memset(cum_all[:, 0, :], 0.0), [], [bcumm[0]])

        def tile_F(ti):
            s_ = ti % 2; d_ = s_
            B0 = 4 * s_
            tsl = slice(ti * 128, (ti + 1) * 128)
            lg = lgs[s_]; top8 = top8s[s_]; idx8 = idx8s[s_]; idxf = idxfs[s_]; blg = blgs[s_]
            gex = gexs[s_]; rksl = rksls[s_]; ohs = ohss[s_]; slotf = slotfs[s_]; brk = brks[s_]
            ld(xin[s_], x1_d[tsl, :], bxin[s_], reads=[bx1ds[ti]])
            for hp in range(2):
                pb = B0 + hp
                for hh in range(2):
                    h = 2 * hp + hh
                    for j in range(2):
                        mm(PS[pb][:, hh * 256:(hh + 1) * 256], qT[:, 2 * h + j, tsl], kT[:, 2 * h + j, :], j == 0, j == 1, [bqT, bkT], bPS[pb])
                yield
                dve(lambda e, hp=hp, pb=pb, d_=d_: e.tensor_reduce(out=nmx[d_][:, 2 * hp:2 * hp + 2], in_=PS[pb][:, :].rearrange("p (a b) -> p a b", a=2), axis=AX.X, op=ALU.max, negate=True),
                    [bPS[pb]], [bnm[d_]])
                for hh in range(2):
                    h = 2 * hp + hh
                    act(lambda e, pb=pb, hh=hh, h=h, d_=d_: e.activation(out=pexp[d_][:, h, :], in_=PS[pb][:, hh * 256:(hh + 1) * 256], func=AF.Exp, bias=nmx[d_][:, h:h + 1], scale=1.0,
                                                                       accum_out=nmx[d_][:, 4 + h:5 + h]), [bPS[pb], bnm[d_]], [bpe[d_], bnm[d_]])
            yield
            dve(lambda e, d_=d_: e.reciprocal(out=nmx[d_][:, 8:12], in_=nmx[d_][:, 4:8]), [bnm[d_]], [bnm[d_]])
            dve(lambda e, d_=d_: e.tensor_mul(out=pexp[d_], in0=pexp[d_], in1=nmx[d_][:, 8:12].unsqueeze(2).broadcast_to([128, 4, 256])), [bpe[d_], bnm[d_]], [bpe[d_]])
            yield
            for hp in range(2):
                pj = B0 + 2 + hp
                for hh in range(2):
                    h = 2 * hp + hh
                    for mt in range(2):
                        tr(PS[pj][:, (hh * 2 + mt) * 128:(hh * 2 + mt + 1) * 128], pexp[d_][:, h, mt * 128:(mt + 1) * 128], ident, [bpe[d_], bconst], bPS[pj])
                o_ = pT[d_][:, hp * 4:hp * 4 + 4, :]
                yield
                if hp == 0:
                    act(lambda e, pj=pj, o_=o_: e.copy(out=o_, in_=PS[pj][:, :].rearrange("p (a b) -> p a b", a=4)), [bPS[pj]], [bpT[d_]])
                else:
                    dve(lambda e, pj=pj, o_=o_: e.tensor_copy(out=o_, in_=PS[pj][:, :].rearrange("p (a b) -> p a b", a=4)), [bPS[pj]], [bpT[d_]])
            yield
            for hp in range(2):
                po = B0 + hp
                for hh in range(2):
                    h = 2 * hp + hh
                    for j in range(2):
                        fc = 2 * h + j
                        for mt in range(2):
                            mm(PS[po][:, (hh * 2 + j) * 128:(hh * 2 + j + 1) * 128], v_m[:, mt, fc * 128:(fc + 1) * 128], pT[d_][:, h * 2 + mt, :], mt == 0, mt == 1, [bvm, bpT[d_]], bPS[po])
                o_ = oT[d_][:, hp * 4:hp * 4 + 4, :]
                yield
                if hp == 0:
                    dve(lambda e, po=po, o_=o_: e.tensor_copy(out=o_, in_=PS[po][:, :].rearrange("p (a b) -> p a b", a=4)), [bPS[po]], [boT[d_]])
                else:
                    act(lambda e, po=po, o_=o_: e.copy(out=o_, in_=PS[po][:, :].rearrange("p (a b) -> p a b", a=4)), [bPS[po]], [boT[d_]])
            yield
            for half in range(2):
                pi = B0 + 2 + half
                for fc in range(8):
                    mm(PS[pi][:, :], oT[d_][:, fc, :], wbig[0][:, fc, half * 512:(half + 1) * 512], fc == 0, fc == 7, [boT[d_], bwbig[0]], bPS[pi])
                yield
                dve(lambda e, s_=s_, pi=pi, half=half: e.scalar_tensor_tensor(out=rt[s_][:, half * 512:(half + 1) * 512], in0=xin[s_][:, half * 512:(half + 1) * 512], scalar=ALPHA,
                                                                         in1=PS[pi][:, :], op0=ALU.mult, op1=ALU.add), [bxin[s_], bPS[pi]], [brt[s_]])
            yield
            layernorm(rt[s_], brt[s_], ln2[0], ln2[1], ln2[2], xo[s_], bxo[s_], par=s_)
            yield
            ld(x2_d[tsl, :], xo[s_], bx2ds[ti], reads=[bxo[s_]])
            act(lambda e, s_=s_: e.copy(out=xob[s_], in_=xo[s_]), [bxo[s_]], [bxob[s_]])
            for half in range(2):
                pi = B0 + half
                for j in range(4):
                    kc = half * 4 + j
                    tr(PS[pi][:, j * 128:(j + 1) * 128], xo[s_][:, kc * 128:(kc + 1) * 128], ident, [bxo[s_], bconst], bPS[pi])
                o = x2T[s_][:, half * 4:half * 4 + 4, :]
                yield
                act(lambda e, o=o, pi=pi: e.copy(out=o, in_=PS[pi][:, :].rearrange("p (a b) -> p a b", a=4)), [bPS[pi]], [bx2T[s_]])
            yield
            pl = B0 + 2; pr = B0 + 3
            for kc in range(8):
                mm(PS[pl][:, 0:NE], x2T[s_][:, kc, :], wr32[:, kc, :], kc == 0, kc == 7, [bx2T[s_], bwr], bPS[pl])
            yield
            dve(lambda e: e.tensor_add(out=lg, in0=PS[pl][:, 0:NE], in1=brow), [bPS[pl], bconst], [blg])
            dve(lambda e: e.max(out=top8, in_=lg), [blg], [blg])
            dve(lambda e: e.max_index(out=idx8, in_max=top8, in_values=lg), [blg], [blg])
            dve(lambda e: e.tensor_copy(out=idxf, in_=idx8[:, 0:4]), [blg], [blg])
            dve(lambda e, ti=ti: e.tensor_scalar(out=mask_all[:, ti, :], in0=lg, scalar1=top8[:, 3:4], scalar2=None, op0=ALU.is_ge), [blg], [bmask[ti]])
            yield
            dve(lambda e: e.tensor_scalar(out=gex[:, 4:5], in0=top8[:, 0:1], scalar1=-1.0, scalar2=None, op0=ALU.mult), [blg], [blg])
            act(lambda e: e.activation(out=gex[:, 0:4], in_=top8[:, 0:4], func=AF.Exp, bias=gex[:, 4:5], scale=1.0, accum_out=gex[:, 5:6]), [blg], [blg])
            yield
            dve(lambda e: e.reciprocal(out=gex[:, 5:6], in_=gex[:, 5:6]), [blg], [blg])
            dve(lambda e, ti=ti: e.tensor_scalar(out=gates_all[:, ti, :], in0=gex[:, 0:4], scalar1=gex[:, 5:6], scalar2=None, op0=ALU.mult), [blg], [bgate[ti]])
            if ti + 1 < 16:
                dve(lambda e, ti=ti: e.tensor_add(out=cum_all[:, ti + 1, :], in0=cum_all[:, ti, :], in1=mask_all[:, ti, :]), [bcumm[ti], bmask[ti]], [bcumm[ti + 1]])
            mm(PS[pr][:, 0:NE], ones, cum_all[:, ti, :], True, False, [bcumm[ti], bconst], bPS[pr])
            mm(PS[pr][:, 0:NE], ustr, mask_all[:, ti, :], False, True, [bmask[ti], bconst], bPS[pr])
            yield
            dve(lambda e: e.tensor_scalar(out=ohs, in0=PS[pr][:, 0:NE], scalar1=float(CAP), scalar2=1.0e6, op0=ALU.is_ge, op1=ALU.mult), [bPS[pr]], [brk])
            dve(lambda e: e.tensor_add(out=rksl, in0=PS[pr][:, 0:NE], in1=base_e), [bPS[pr], bconst], [brk])
            dve(lambda e: e.tensor_add(out=rksl, in0=rksl, in1=ohs), [brk], [brk])
            for k in range(4):
                dve(lambda e, k=k: e.scalar_tensor_tensor(out=ohs, in0=iota_e, scalar=idxf[:, k:k + 1], in1=rksl, op0=ALU.is_equal, op1=ALU.mult, accum_out=slotf[:, k:k + 1]),
                    [brk, blg, bconst], [brk])
            dve(lambda e: e.tensor_scalar(out=slotf, in0=slotf, scalar1=float(NE * CAP), scalar2=None, op0=ALU.min), [brk], [brk])
            dve(lambda e, ti=ti: e.tensor_copy(out=slots_all[:, ti * 4:ti * 4 + 4], in_=slotf), [brk], [bslot[ti]])
            yield
            for k in range(4):
                P.dma("pool", lambda e, ti=ti, k=k, s_=s_: e.indirect_dma_start(out=xbuf_d, out_offset=bass.IndirectOffsetOnAxis(ap=slots_all[:, ti * 4 + k:ti * 4 + k + 1], axis=0),
                                                                                 in_=xob[s_], in_offset=None),
                      [bxob[s_], bslot[ti], bxb], [bxbk[k]])
            yield

        run_lockstep([tile_F(ti) for ti in range(16)])
        P.release()
        P.release()
        P.barrier()

        if stop == "F":
            P.emit()
            return nc
        P.mark()
        b1r = P.sb([32, 2 * D]); bb1r = P.buf("b1r"); b1c = P.sb([128, 16, NE]); bb1c = P.buf("b1c")
        ld(b1r, b1_d, bb1r)
        for g4 in range(4):
            pi = g4 % 2
            for j in range(4):
                fc = g4 * 4 + j
                tr(PS[pi][:, j * NE:(j + 1) * NE], b1r[:, fc * 128:(fc + 1) * 128], ident[0:32, 0:32], [bb1r, bconst], bPS[pi])
            dve(lambda e, pi=pi, g4=g4: e.tensor_copy(out=b1c[:, g4 * 4:g4 * 4 + 4, :], in_=PS[pi][:, 0:4 * NE].rearrange("p (a b) -> p a b", a=4)), [bPS[pi]], [bb1c])
        xbT = P.sb([128, 8, CAP], BF16); bxbT = P.buf("xbT")
        w1e = [P.sb([128, 8, 2 * D], BF16) for _ in range(2)]; bw1e = [P.buf(f"w1e{i}") for i in range(2)]
        w2e = [P.sb([128, 8, D], BF16) for _ in range(2)]; bw2e = [P.buf(f"w2e{i}") for i in range(2)]
        xb2 = [P.sb([128, 3, D], BF16) for _ in range(2)]; bxb2 = [P.buf(f"xb2{i}") for i in range(2)]
        identb2 = P.sb([128, 128], BF16); bidb = P.buf("identb2")
        act(lambda e: e.copy(out=identb2, in_=ident), [bconst], [bidb])
        actT = P.sb([128, 8, CAP], BF16); bact = P.buf("actT")
        y_sb = [P.sb([128, 3, D])] * 2; by = [P.buf("y0")] * 2
        b2row = [P.sb([128, D]) for _ in range(2)]; bb2 = [P.buf(f"b2r{i}") for i in range(2)]
        g1 = [P.sb([128, CAP]) for _ in range(2)]; l0 = [P.sb([128, CAP]) for _ in range(2)]; sg = [P.sb([128, CAP]) for _ in range(2)]
        bg1 = [P.buf(f"g1{i}") for i in range(2)]; bl0 = [P.buf(f"l0{i}") for i in range(2)]; bsg = [P.buf(f"sg{i}") for i in range(2)]
        w1_v = w1_d.rearrange("e (c p) n -> e p c n", p=128)
        w2_v = w2_d.rearrange("e (c p) n -> e p c n", p=128)

        def load_w1(e_):
            ld(w1e[e_ % 2], w1_v[e_], bw1e[e_ % 2], q="pool")

        def load_w2(e_):
            ld(w2e[e_ % 2], w2_v[e_], bw2e[e_ % 2], q="pool")
            ld(b2row[e_ % 2], b2_d[e_:e_ + 1, :].broadcast_to([128, D]), bb2[e_ % 2])

        def load_x(e_):
            ld(xb2[e_ % 2], xbuf_d[e_ * CAP:(e_ + 1) * CAP, :].rearrange("(a p) d -> p a d", p=128), bxb2[e_ % 2], reads=[bxb] + bxbk)

        load_x(0)
        load_w1(0)
        load_w2(0)
        load_x(1)
        load_w1(1)
        load_w2(1)
        for e_ in range(NE):
            s_ = e_ % 2
            for kc in range(8):
                pi = kc % 2
                for a in range(3):
                    tr(PS[pi][:, :].bitcast(BF16)[:, a * 128:(a + 1) * 128], xb2[s_][:, a, kc * 128:(kc + 1) * 128], identb2, [bxb2[s_], bidb], bPS[pi])
                if kc % 2 == 0:
                    act(lambda e, pi=pi, kc=kc: e.copy(out=xbT[:, kc, :], in_=PS[pi][:, :].bitcast(BF16)[:, 0:CAP]), [bPS[pi]], [bxbT])
                else:
                    dve(lambda e, pi=pi, kc=kc: e.tensor_copy(out=xbT[:, kc, :], in_=PS[pi][:, :].bitcast(BF16)[:, 0:CAP]), [bPS[pi]], [bxbT])
            if e_ + 2 < NE:
                load_x(e_ + 2)
            for pb in range(2):
                for cc in range(4):
                    fc = pb * 4 + cc
                    r_ = fc % 2
                    pa = 2 + r_; pl = 4 + r_
                    for kc in range(8):
                        mm(PS[pa][:, 0:CAP], w1e[s_][:, kc, fc * 128:(fc + 1) * 128], xbT[:, kc, :], kc == 0, kc == 7, [bw1e[s_], bxbT], bPS[pa])
                    for kc in range(8):
                        mm(PS[pl][:, 0:CAP], w1e[s_][:, kc, D + fc * 128:D + (fc + 1) * 128], xbT[:, kc, :], kc == 0, kc == 7, [bw1e[s_], bxbT], bPS[pl])
                    dve(lambda e, r_=r_, pa=pa, fc=fc, e_=e_: e.tensor_scalar(out=g1[r_], in0=PS[pa][:, 0:CAP], scalar1=b1c[:, fc, e_:e_ + 1], scalar2=7.0, op0=ALU.add, op1=ALU.min),
                        [bPS[pa], bb1c], [bg1[r_]])
                    act(lambda e, r_=r_, pl=pl, fc=fc, e_=e_: e.activation(out=l0[r_], in_=PS[pl][:, 0:CAP], func=AF.Identity, bias=b1c[:, 8 + fc, e_:e_ + 1], scale=1.0),
                        [bPS[pl], bb1c], [bl0[r_]])
                    act(lambda e, r_=r_: e.activation(out=sg[r_], in_=g1[r_], func=AF.Sigmoid, scale=1.702), [bg1[r_]], [bsg[r_]])
                    dve(lambda e, r_=r_: e.tensor_scalar(out=l0[r_], in0=l0[r_], scalar1=7.0, scalar2=-7.0, op0=ALU.min, op1=ALU.max), [bl0[r_]], [bl0[r_]])
                    dve(lambda e, r_=r_: e.tensor_mul(out=g1[r_], in0=g1[r_], in1=sg[r_]), [bg1[r_], bsg[r_]], [bg1[r_]])
                    dve(lambda e, r_=r_, fc=fc: e.scalar_tensor_tensor(out=actT[:, fc, :], in0=l0[r_], scalar=1.0, in1=g1[r_], op0=ALU.add, op1=ALU.mult), [bl0[r_], bg1[r_]], [bact])
            if e_ + 2 < NE:
                load_w1(e_ + 2)
            for half in range(2):
                for a in range(3):
                    pi = 6 + (half * 3 + a) % 2
                    for fc in range(8):
                        mm(PS[pi][:, :], actT[:, fc, a * 128:(a + 1) * 128], w2e[s_][:, fc, half * 512:(half + 1) * 512], fc == 0, fc == 7, [bact, bw2e[s_]], bPS[pi])
                    dve(lambda e, pi=pi, a=a, half=half, s_=s_: e.tensor_add(out=y_sb[0][:, a, half * 512:(half + 1) * 512], in0=PS[pi][:, :], in1=b2row[s_][:, half * 512:(half + 1) * 512]),
                        [bPS[pi], bb2[s_]], [by[0]])
            if e_ + 2 < NE:
                load_w2(e_ + 2)
            ld(ybuf_d[e_ * CAP:(e_ + 1) * CAP, :].rearrange("(a p) d -> p a d", p=128), y_sb[0], byb, reads=[by[0]])
        P.release()
        P.barrier()

        P.mark()
        gath = [P.sb([128, D]) for _ in range(12)]; bga = [P.buf(f"ga{i}") for i in range(12)]
        xin3 = [P.sb([128, D]) for _ in range(3)]; bxin3 = [P.buf(f"xin3{i}") for i in range(3)]
        ln3 = load_ln(2)
        rt = [P.sb([128, D]) for _ in range(2)]; brt = [P.buf(f"rtc{i}") for i in range(2)]
        xo = [P.sb([128, D]) for _ in range(2)]; bxo = [P.buf(f"xoc{i}") for i in range(2)]
        bout = P.buf("out")
        def comb_load(ti):
            g3 = ti % 3
            ld(xin3[g3], x2_d[ti * 128:(ti + 1) * 128, :], bxin3[g3], reads=[bx2ds[ti]])
            for k in range(4):
                P.dma("pool", lambda e, ti=ti, k=k, g3=g3: e.indirect_dma_start(out=gath[g3 * 4 + k], out_offset=None, in_=ybuf_d,
                                                                                 in_offset=bass.IndirectOffsetOnAxis(ap=slots_all[:, ti * 4 + k:ti * 4 + k + 1], axis=0)),
                      [byb, bybz, bslot[ti]], [bga[g3 * 4 + k]])

        comb_load(0)
        comb_load(1)
        for ti in range(16):
            s_ = ti % 2
            g3 = ti % 3
            tsl = slice(ti * 128, (ti + 1) * 128)
            if ti + 2 < 16:
                comb_load(ti + 2)
            act(lambda e, s_=s_, g3=g3: e.activation(out=rt[s_], in_=xin3[g3], func=AF.Copy, scale=ALPHA), [bxin3[g3]], [brt[s_]])
            for k in range(4):
                dve(lambda e, s_=s_, k=k, ti=ti, g3=g3: e.scalar_tensor_tensor(out=rt[s_], in0=gath[g3 * 4 + k], scalar=gates_all[:, ti, k:k + 1], in1=rt[s_], op0=ALU.mult, op1=ALU.add),
                    [bga[g3 * 4 + k], bgate[ti], brt[s_]], [brt[s_]])
            layernorm(rt[s_], brt[s_], ln3[0], ln3[1], ln3[2], xo[s_], bxo[s_], par=s_, norm_on_act=True)
            ld(out_d[tsl, :], xo[s_], bout, reads=[bxo[s_]])
        P.release()
        P.barrier()
        P.emit()
    return nc


_NC_CACHE = {}


def _consts(core):
    s_ = np.arange(64)[:, None]; t_ = np.arange(64)[None, :]
    m2 = (s_ <= t_).astype(np.float32)
    m1 = m2 - (s_ <= 31).astype(np.float32)
    m3 = (s_ > t_).astype(np.float32)
    a = np.arange(128)
    inv16 = np.zeros((4, 16), np.float32)
    for g, w in enumerate((2, 4, 8, 16)):
        for j in range(16):
            inv16[g, j] = 1.0 / (min(j + 1, w) if core == 0 else w)
    return {
        "c_ident": np.eye(128, dtype=np.float32),
        "c_m12": np.ascontiguousarray(np.concatenate([m1, m2], axis=1)),
        "c_m3": m3,
        "c_ustr": (a[:, None] < a[None, :]).astype(np.float32),
        "c_ones": np.ones((128, 128), np.float32),
        "c_iota": np.ascontiguousarray(np.broadcast_to(np.arange(NE, dtype=np.float32), (128, NE))),
        "c_base": np.ascontiguousarray(np.broadcast_to(np.arange(NE, dtype=np.float32) * CAP, (128, NE))),
        "c_inv16": np.ascontiguousarray(np.broadcast_to(inv16.reshape(1, 64), (128, 64))),
    }


def kernel(**inputs):
    debug = bool(os.environ.get("MK_DEBUG"))
    f = lambda k: np.ascontiguousarray(np.asarray(inputs[k], dtype=np.float32))
    x = f("x")[0]
    shared = {
        "mem": f("mem")[0], "w_in": f("w_in")[0], "lb_logits": f("lb_logits"), "hgrn_norm_w": f("hgrn_norm_w"),
        "w_pool": f("w_pool")[0], "pool_scale": f("pool_scale").reshape(4, 128), "w_out": f("w_out")[0],
        "ln1_w": f("ln1_w"), "ln1_b": f("ln1_b"), "ln2_w": f("ln2_w"), "ln2_b": f("ln2_b"), "ln3_w": f("ln3_w"), "ln3_b": f("ln3_b"),
        "w_xq": f("w_xq")[0], "w_xk": f("w_xk")[0], "w_xv": f("w_xv")[0], "w_xo": f("w_xo")[0],
        "w_router": f("w_router")[0], "b_router": f("b_router"),
        "w1": f("w1")[0], "b1": f("b1")[0], "w2": f("w2")[0], "b2": f("b2")[0],
    }
    if debug not in _NC_CACHE:
        _NC_CACHE[debug] = build_program(debug)
    nc = _NC_CACHE[debug]
    in_maps = []
    for c in range(NCORES):
        m = dict(shared)
        m["x"] = np.ascontiguousarray(x[c * NT:(c + 1) * NT])
        m["xw"] = np.ascontiguousarray(x[c * NT - W:c * NT]) if c > 0 else np.zeros((W, D), np.float32)
        m.update(_consts(c))
        in_maps.append(m)
    res = run_bass_kernel_spmd(nc, in_maps, core_ids=list(range(NCORES)))
    out = np.concatenate([r["out"] for r in res.results], axis=0)[None]
    if debug:
        kernel.dbg = {k: np.concatenate([r[k] for r in res.results], axis=0) for k in ("x1s", "x2s")}
    return out.astype(np.float32)
```
